# Optimizing a Trainium2 kernel written in Bass

```python
import jax, jax.numpy as jnp
from jax import lax
import numpy as np

D_MODEL = 1024
BATCH = 16
SEQ = 2048
DEPTH = 1

D_MIX = D_MODEL
D_CONV = D_MIX // 2
D_RET = D_MIX - D_CONV
N_RET_HEADS = 4
RET_HEAD_DIM = D_RET // N_RET_HEADS
CONV_WIDTH = 31
CHUNK = 128
D_FF = ((8 * D_MODEL // 3 + 255) // 256) * 256
D_PLE = 256
ROPE_BASE = 10000.0
EPS = 1e-6
D_IN = 2 * D_CONV + 4 * D_RET

kernel_name = "hymba_conformer_retention_block"


def rmsnorm(x, w):
    xf = x.astype(jnp.float32)
    y = xf * lax.rsqrt(jnp.mean(xf * xf, axis=-1, keepdims=True) + EPS)
    return (y * w.astype(jnp.float32)).astype(x.dtype)


def layernorm(x, w, b):
    xf = x.astype(jnp.float32)
    mu = jnp.mean(xf, axis=-1, keepdims=True)
    var = jnp.mean(jnp.square(xf - mu), axis=-1, keepdims=True)
    y = (xf - mu) * lax.rsqrt(var + EPS)
    return (y * w.astype(jnp.float32) + b.astype(jnp.float32)).astype(x.dtype)


def rotary(t, pos):
    half = t.shape[-1] // 2
    freqs = ROPE_BASE ** (-jnp.arange(half, dtype=jnp.float32) / half)
    ang = pos[:, None] * freqs[None, :]
    cos = jnp.cos(ang)[None, :, None, :]
    sin = jnp.sin(ang)[None, :, None, :]
    t1, t2 = t[..., :half], t[..., half:]
    return jnp.concatenate([t1 * cos - t2 * sin, t1 * sin + t2 * cos], axis=-1)


def conformer_conv(u_a, u_b, dw_w, dw_b, ln_w, ln_b):
    u = u_a * jax.nn.sigmoid(u_b)
    y = lax.conv_general_dilated(
        u, dw_w[:, None, :].astype(u.dtype), window_strides=(1,),
        padding=[(CONV_WIDTH - 1, 0)],
        dimension_numbers=("NWC", "WIO", "NWC"),
        feature_group_count=D_CONV)
    y = y + dw_b
    y = layernorm(y, ln_w, ln_b)
    return jax.nn.silu(y)


def chunkwise_retention(q, k, v):
    b, s, h, dh = q.shape
    nc = s // CHUNK
    log_g = jnp.log(1.0 - 2.0 ** (-5.0 - jnp.arange(h, dtype=jnp.float32)))
    idx = jnp.arange(CHUNK, dtype=jnp.float32)
    rel = idx[:, None] - idx[None, :]
    decay_in = jnp.where(rel >= 0, jnp.exp(log_g[:, None, None] * jnp.maximum(rel, 0.0)), 0.0)
    q_decay = jnp.exp(log_g[:, None] * (idx + 1.0))[None, :, :, None]
    k_decay = jnp.exp(log_g[:, None] * (CHUNK - 1.0 - idx))[None, :, :, None]
    chunk_decay = jnp.exp(log_g * CHUNK)[None, :, None, None]

    def to_chunks(t):
        return t.reshape(b, nc, CHUNK, h, dh).transpose(1, 0, 3, 2, 4)

    def step(state, qkv):
        qc, kc, vc = qkv
        scores = jnp.einsum("bhid,bhjd->bhij", qc, kc) * decay_in[None]
        inner = jnp.einsum("bhij,bhjd->bhid", scores, vc)
        cross = jnp.einsum("bhid,bhde->bhie", qc, state) * q_decay
        new_state = state * chunk_decay + jnp.einsum("bhjd,bhje->bhde", kc * k_decay, vc)
        return new_state, inner + cross

    state0 = jnp.zeros((b, h, dh, dh), jnp.float32)
    _, out = lax.scan(step, state0, (to_chunks(q), to_chunks(k), to_chunks(v)))
    return out.transpose(1, 0, 3, 2, 4).reshape(b, s, h, dh)


def retention_group(hq, hk, hv, hg):
    b, s, _ = hq.shape
    shp = (b, s, N_RET_HEADS, RET_HEAD_DIM)
    pos = jnp.arange(s, dtype=jnp.float32)
    q = rotary(hq.reshape(shp).astype(jnp.float32), pos)
    k = rotary(hk.reshape(shp).astype(jnp.float32), pos) * (RET_HEAD_DIM ** -0.5)
    v = hv.reshape(shp).astype(jnp.float32)
    o = chunkwise_retention(q, k, v)
    o = o * lax.rsqrt(jnp.mean(o * o, axis=-1, keepdims=True) + EPS)
    o = o.reshape(b, s, D_RET).astype(hg.dtype)
    return jax.nn.silu(hg) * o


def setup_inputs(seed: int = 0) -> dict:
    key = jax.random.key(seed)
    ks = jax.random.split(key, 20)
    f32 = jnp.float32

    def nrm(k, shape, scale):
        return jax.random.normal(k, shape, f32) * scale

    def gain(k, shape):
        return 1.0 + 0.01 * jax.random.normal(k, shape, f32)

    L = DEPTH
    return {
        "x": nrm(ks[0], (BATCH, SEQ, D_MODEL), 1.0),
        "p": nrm(ks[1], (DEPTH, BATCH, SEQ, D_PLE), 1.0),
        "mix_pre_norm": gain(ks[2], (L, D_MODEL)),
        "w_in": nrm(ks[3], (L, D_MODEL, D_IN), D_MODEL ** -0.5),
        "conv_dw_w": nrm(ks[4], (L, CONV_WIDTH, D_CONV), CONV_WIDTH ** -0.5),
        "conv_dw_b": nrm(ks[5], (L, D_CONV), 0.01),
        "conv_ln_w": gain(ks[6], (L, D_CONV)),
        "conv_ln_b": nrm(ks[7], (L, D_CONV), 0.01),
        "w_out": nrm(ks[8], (L, D_MIX, D_MODEL), D_MIX ** -0.5),
        "mix_post_norm": gain(ks[9], (L, D_MODEL)),
        "ffn_pre_norm": gain(ks[10], (L, D_MODEL)),
        "w_ffn_gate": nrm(ks[11], (L, D_MODEL, D_FF), D_MODEL ** -0.5),
        "w_ffn_up": nrm(ks[12], (L, D_MODEL, D_FF), D_MODEL ** -0.5),
        "w_ffn_down": nrm(ks[13], (L, D_FF, D_MODEL), D_FF ** -0.5),
        "ffn_post_norm": gain(ks[14], (L, D_MODEL)),
        "w_ple_gate": nrm(ks[15], (L, D_MODEL, D_MODEL), D_MODEL ** -0.5),
        "w_ple_proj": nrm(ks[16], (L, D_PLE, D_MODEL), D_PLE ** -0.5),
        "ple_post_norm": gain(ks[17], (L, D_MODEL)),
    }


def reference(x, p, mix_pre_norm, w_in, conv_dw_w, conv_dw_b, conv_ln_w, conv_ln_b,
              w_out, mix_post_norm, ffn_pre_norm, w_ffn_gate, w_ffn_up, w_ffn_down,
              ffn_post_norm, w_ple_gate, w_ple_proj, ple_post_norm):
    for i in range(DEPTH):
        h = rmsnorm(x, mix_pre_norm[i])
        u = h @ w_in[i]
        c0, c1 = D_CONV, 2 * D_CONV
        u_a, u_b = u[..., :c0], u[..., c0:c1]
        hq = u[..., c1:c1 + D_RET]
        hk = u[..., c1 + D_RET:c1 + 2 * D_RET]
        hv = u[..., c1 + 2 * D_RET:c1 + 3 * D_RET]
        hg = u[..., c1 + 3 * D_RET:]
        y_conv = conformer_conv(u_a, u_b, conv_dw_w[i], conv_dw_b[i], conv_ln_w[i], conv_ln_b[i])
        y_ret = retention_group(hq, hk, hv, hg)
        y = jnp.concatenate([y_conv, y_ret], axis=-1) @ w_out[i]
        x = x + rmsnorm(y, mix_post_norm[i])
        h = rmsnorm(x, ffn_pre_norm[i])
        f = (jax.nn.silu(h @ w_ffn_gate[i]) * (h @ w_ffn_up[i])) @ w_ffn_down[i]
        x = x + rmsnorm(f, ffn_post_norm[i])
        e = jax.nn.sigmoid(x @ w_ple_gate[i]) * (p[i].astype(x.dtype) @ w_ple_proj[i])
        x = x + rmsnorm(e, ple_post_norm[i])
    return x
```

```python
import numpy as np
from contextlib import ExitStack
import concourse.bass as bass
import concourse.mybir as mybir
from concourse.bass_utils import run_bass_kernel_spmd

F32 = mybir.dt.float32
BF16 = mybir.dt.bfloat16
ALU = mybir.AluOpType
AF = mybir.ActivationFunctionType

ENGS = ("pe", "act", "dve", "pool", "sp")
EPS = 1e-6
NCORES = 8
TOK_CORE = 4096
GT = 1024
NG = TOK_CORE // GT
DFF = 2816
NFC = DFF // 128


class Tok:
    __slots__ = ("name", "w", "rs", "lo", "hi")

    def __init__(self, name="", lo=None, hi=None):
        self.name = name
        self.w = None
        self.rs = []
        self.lo = lo
        self.hi = hi


class Op:
    __slots__ = ("eng", "fn", "deps", "signal", "semval", "idx", "stream")

    def __init__(self, eng, fn, stream):
        self.eng = eng
        self.fn = fn
        self.stream = stream
        self.deps = []
        self.signal = False
        self.semval = 0
        self.idx = 0


class Prog:
    SAME_ENG_WINDOW = 10 ** 9

    def __init__(self):
        self.q = {e: [] for e in ENGS}
        self.stream_cnt = {}
        self.rtoks = []

    def tok(self, name, lo=None, hi=None):
        t = Tok(name, lo, hi)
        if lo is not None:
            self.rtoks.append(t)
        return t

    def _alias(self, t):
        if t.lo is None:
            return ()
        return [u for u in self.rtoks if u is not t and u.lo < t.hi and t.lo < u.hi]

    def op(self, eng, fn, reads=(), writes=(), stream=None):
        o = Op(eng, fn, stream)
        o.idx = len(self.q[eng])
        deps = {}
        for t in reads:
            if t.w is not None:
                deps[id(t.w)] = t.w
            for u in self._alias(t):
                if u.w is not None:
                    deps[id(u.w)] = u.w
        for t in writes:
            for u in [t] + list(self._alias(t)):
                if u.w is not None:
                    deps[id(u.w)] = u.w
                for r in u.rs:
                    deps[id(r)] = r
        for d in deps.values():
            if d is o:
                continue
            if d.stream is None and d.eng == eng and stream is None:
                if eng == "pe":
                    continue
                if o.idx - d.idx > self.SAME_ENG_WINDOW:
                    continue
            if d.stream is None:
                d.signal = True
            o.deps.append(d)
        for t in reads:
            t.rs.append(o)
        for t in writes:
            t.w = o
            t.rs = []
        if stream is not None:
            c = self.stream_cnt.get(stream, 0) + 1
            self.stream_cnt[stream] = c
            o.semval = 16 * c
        self.q[eng].append(o)
        return o

    def emit(self, nc, es, final_streams=("out",)):
        sems = {e: es.enter_context(nc.semaphore("s_" + e)) for e in ENGS if e != "sp"}
        ssems = {s: es.enter_context(nc.semaphore("d_" + s)) for s in self.stream_cnt}
        for e in ENGS:
            c = 0
            for o in self.q[e]:
                if o.stream is None and o.signal:
                    c += 1
                    o.semval = c
        block = es.enter_context(nc.Block())

        def run(ename, eng):
            waited = {}
            for o in self.q[ename]:
                need = {}
                for d in o.deps:
                    key = ("s", d.stream) if d.stream is not None else ("e", d.eng)
                    if d.semval > need.get(key, 0):
                        need[key] = d.semval
                for key, val in need.items():
                    if waited.get(key, 0) >= val:
                        continue
                    sem = ssems[key[1]] if key[0] == "s" else sems[key[1]]
                    eng.wait_ge(sem, val)
                    waited[key] = val
                ins = o.fn(eng)
                if o.stream is not None:
                    ins.then_inc(ssems[o.stream], 16)
                elif o.signal:
                    ins.then_inc(sems[ename], 1)
            if ename == "sp":
                for s in self.stream_cnt:
                    if s.startswith("out") or s.startswith("dbg"):
                        eng.wait_ge(ssems[s], 16 * self.stream_cnt[s])

        @block.tensor
        def _(eng):
            run("pe", eng)

        @block.scalar
        def _(eng):
            run("act", eng)

        @block.vector
        def _(eng):
            run("dve", eng)

        @block.gpsimd
        def _(eng):
            run("pool", eng)

        @block.sync
        def _(eng):
            run("sp", eng)


class _Stop(Exception):
    pass


def build_program(ngroups=NG, dbg=None, stop=None, dbg_group=0):
    nc = bass.Bass("TRN2", target_bir_lowering=False)
    dbg = dbg or ()

    def din(name, shape):
        return nc.dram_tensor(name, shape, F32, kind="ExternalInput").ap()

    x = din("x", [TOK_CORE, 1024])
    p = din("p", [TOK_CORE, 256])
    w_in = din("w_in", [1024, 3072])
    w_out = din("w_out", [1024, 1024])
    w_gate = din("w_gate", [1024, DFF])
    w_up = din("w_up", [1024, DFF])
    w_down = din("w_down", [DFF, 1024])
    w_pg = din("w_pg", [1024, 1024])
    w_pp = din("w_pp", [256, 1024])
    g_pre = din("g_pre", [1024])
    g_post = din("g_post", [1024])
    g_fpre = din("g_fpre", [1024])
    g_fpost = din("g_fpost", [1024])
    g_ple = din("g_ple", [1024])
    dw_w = din("dw_w", [31, 512])
    dw_b = din("dw_b", [512])
    ln_w = din("ln_w", [512])
    ln_b = din("ln_b", [512])
    c_ident = din("c_ident", [128, 128])
    c_cos = din("c_cos", [2048, 128])
    c_sin = din("c_sin", [2048, 128])
    c_dec = din("c_dec", [128, 8])
    c_mask = din("c_mask", [128, 512])
    out = nc.dram_tensor("out", [TOK_CORE, 1024], F32, kind="ExternalOutput").ap()

    g128 = [float((1.0 - 2.0 ** (-5.0 - h)) ** 128) for h in range(4)]

    es = ExitStack()
    P = Prog()
    with es:
        def sb(name, shape, dt):
            return es.enter_context(nc.sbuf_tensor(name, shape, dt))

        xt = sb("xt", [128, 8, 1024], F32)
        T_xt = [P.tok("xt%d" % i) for i in range(8)]
        hT = sb("hT", [128, 8, GT], BF16)
        T_hT = [P.tok("hT%d" % i) for i in range(8)]
        cosb = sb("cosb", [128, 8, 128], F32)
        sinb = sb("sinb", [128, 8, 128], F32)
        T_cos = P.tok("cos")
        T_sin = P.tok("sin")
        maskb = sb("maskb", [128, 512], F32)
        gbc = sb("gbc", [128, 1024], F32)
        T_gbc = P.tok("gbc")
        identf = sb("identf", [128, 128], F32)
        identb = sb("identb", [128, 128], BF16)
        ones_s = sb("ones_s", [128, 128], BF16)
        nhalf = sb("nhalf", [128, 512], F32)
        epsb = sb("epsb", [128, 8], F32)
        decb = sb("decb", [128, 8], F32)
        gpre = sb("gpre", [128, 8], F32)
        gfpre = sb("gfpre", [128, 8], F32)
        cw = sb("cw", [128, 4, 31], F32)
        cb = sb("cb", [128, 4], F32)
        lnw = sb("lnw", [128, 4], F32)
        lnb = sb("lnb", [128, 4], F32)
        T_const = P.tok("const")
        halo_sv = sb("halo_sv", [128, 4, 30], BF16)
        T_halo = P.tok("halo_sv")
        S32 = sb("S32", [128, 512], F32)
        Sbf = sb("Sbf", [128, 512], BF16)
        T_S32 = P.tok("S32")
        T_Sbf = P.tok("Sbf")
        xn = [sb("xn%d" % i, [128, 1024], BF16) for i in range(2)]
        T_xn = [P.tok("xn%d" % i) for i in range(2)]
        junk = sb("junk", [128, 1024], BF16)
        T_junk = P.tok("junk")
        tmp32 = [sb("tmp32_%d" % i, [128, 512], F32) for i in range(4)]
        T_tmp32 = [P.tok("tmp32_%d" % i) for i in range(4)]
        NSM = 48
        sm = [sb("sm%d" % i, [128, 16], F32) for i in range(NSM)]
        T_sm = [P.tok("sm%d" % i) for i in range(NSM)]

        UBYTES = 120 * 1024
        U = sb("U", [128, UBYTES // 2], BF16)

        def carve(off, nbytes, dt):
            assert off % 4 == 0 and off + nbytes <= UBYTES
            v = U[:, off // 2:(off + nbytes) // 2]
            if dt == F32:
                v = v.bitcast(F32)
            return v

        K = 1024
        aT = carve(0, 45056, BF16).rearrange("p (c t) -> p c t", c=NFC)
        T_a = [[P.tok("a%d_%d" % (c, th), c * 2048 + th * 1024, c * 2048 + th * 1024 + 1024) for th in range(2)]
               for c in range(NFC)]
        wd = carve(45056, 45056, BF16).rearrange("p (c n) -> p c n", c=NFC)
        T_wd = [P.tok("wd%d" % j, 45056 + j * 22528, 45056 + (j + 1) * 22528) for j in range(2)]
        WG0 = 90112
        wgr = [carve(WG0 + i * 4096, 4096, BF16).rearrange("p (k n) -> p k n", k=8) for i in range(2)]
        wur = [carve(WG0 + 8192 + i * 4096, 4096, BF16).rearrange("p (k n) -> p k n", k=8) for i in range(2)]
        T_wgr = [P.tok("wgr%d" % i, WG0 + i * 4096, WG0 + (i + 1) * 4096) for i in range(2)]
        T_wur = [P.tok("wur%d" % i, WG0 + 8192 + i * 4096, WG0 + 8192 + (i + 1) * 4096) for i in range(2)]
        BT0 = WG0 + 16384
        sgt = [carve(BT0 + i * 2048, 2048, F32) for i in range(2)]
        T_sgt = [P.tok("sgt%d" % i, BT0 + i * 2048, BT0 + (i + 1) * 2048) for i in range(2)]
        e32 = [carve(BT0 + 4096, 4096, F32), carve(36864, 4096, F32)]
        T_e32 = [[P.tok("e32_0_%d" % h, BT0 + 4096 + h * 2048, BT0 + 4096 + (h + 1) * 2048) for h in range(2)],
                 [P.tok("e32_1_%d" % h, 36864 + h * 2048, 36864 + (h + 1) * 2048) for h in range(2)]]
        tt32 = [carve(BT0 + 8192, 4096, F32), carve(40960, 4096, F32)]
        T_tt32 = [[P.tok("tt32_0_%d" % h, BT0 + 8192 + h * 2048, BT0 + 8192 + (h + 1) * 2048) for h in range(2)],
                  [P.tok("tt32_1_%d" % h, 40960 + h * 2048, 40960 + (h + 1) * 2048) for h in range(2)]]
        wpg = carve(0, 16384, BF16).rearrange("p (k n) -> p k n", k=8)
        T_wpg = P.tok("wpg", 0, 16384)
        wpp = carve(16384, 4096, BF16).rearrange("p (k n) -> p k n", k=2)
        T_wpp = P.tok("wpp", 16384, 20480)
        pT = carve(20480, 4096, BF16).rearrange("p (k t) -> p k t", k=2)
        T_pT = [[P.tok("pT%d_%d" % (i, k), 20480 + k * 2048 + i * 256, 20480 + k * 2048 + (i + 1) * 256) for k in range(2)]
                for i in range(8)]
        pt32 = [carve(24576 + i * 1024, 1024, F32) for i in range(2)]
        T_pt32 = [P.tok("pt32_%d" % i, 24576 + i * 1024, 24576 + (i + 1) * 1024) for i in range(2)]
        ptb = [carve(26624 + i * 512, 512, BF16) for i in range(2)]
        T_ptb = [P.tok("ptb%d" % i, 26624 + i * 512, 26624 + (i + 1) * 512) for i in range(2)]
        ot = [carve(28672 + i * 4096, 4096, F32) for i in range(2)]
        T_ot = [P.tok("ot%d" % i, 28672 + i * 4096, 28672 + (i + 1) * 4096) for i in range(2)]
        catT = carve(0, 16384, BF16).rearrange("p (k t) -> p k t", k=8)
        T_cat = [[P.tok("cat%d_%d" % (k, j), k * 2048 + j * 256, k * 2048 + (j + 1) * 256) for j in range(8)]
                 for k in range(8)]
        GL0 = 16384
        GLW = 30 + GT
        glu = carve(GL0, 4 * GLW * 2 + 16, BF16)[:, 0:4 * GLW].rearrange("p (c t) -> p c t", c=4)
        T_glu = [[P.tok("glu%d_%d" % (c, j), GL0 + c * GLW * 2 + (0 if j == 0 else 60 + (j - 1) * 1024),
                        GL0 + c * GLW * 2 + (60 if j == 0 else 60 + j * 1024)) for j in range(3)] for c in range(4)]
        YB0 = GL0 + 8448
        ybf = carve(YB0, 4096, BF16).rearrange("p (c t) -> p c t", c=4)
        ysq = carve(YB0 + 4096, 4096, BF16).rearrange("p (c t) -> p c t", c=4)
        T_ybf = [P.tok("ybf%d" % c, YB0 + c * 1024, YB0 + (c + 1) * 1024) for c in range(4)]
        T_ysq = [P.tok("ysq%d" % c, YB0 + 4096 + c * 1024, YB0 + 4096 + (c + 1) * 1024) for c in range(4)]
        AC0 = YB0 + 8192
        acc = [carve(AC0 + c * 2048, 2048, F32) for c in range(4)]
        T_acc = [P.tok("acc%d" % c, AC0 + c * 2048, AC0 + (c + 1) * 2048) for c in range(4)]
        LN0 = AC0 + 8192
        mu_sb = carve(LN0, 2048, F32)
        musq = carve(LN0 + 2048, 2048, F32)
        rstdb = carve(LN0 + 4096, 2048, F32)
        T_mu = P.tok("mu", LN0, LN0 + 2048)
        T_musq = P.tok("musq", LN0 + 2048, LN0 + 4096)
        T_rstdb = P.tok("rstdb", LN0 + 4096, LN0 + 6144)
        assert LN0 + 6144 <= 45056 + 4096
        RG0 = 49152
        ring = [carve(RG0 + i * 8192, 8192, BF16).rearrange("p (k n) -> p k n", k=8) for i in range(2)]
        T_ring = [P.tok("ring%d" % i, RG0 + i * 8192, RG0 + (i + 1) * 8192) for i in range(2)]
        wo = carve(RG0, 16384, BF16).rearrange("p (k n) -> p k n", k=8)
        T_wo = P.tok("wo", RG0, RG0 + 16384)
        ST0 = RG0 + 16384
        qs = carve(ST0, 8192, BF16).rearrange("p (i n) -> p i n", i=8)
        ks = carve(ST0 + 8192, 8192, BF16).rearrange("p (i n) -> p i n", i=8)
        vs = carve(ST0 + 16384, 8192, BF16).rearrange("p (i n) -> p i n", i=8)
        T_qs = [P.tok("qs%d" % i, ST0 + i * 1024, ST0 + (i + 1) * 1024) for i in range(8)]
        T_ks = [P.tok("ks%d" % i, ST0 + 8192 + i * 1024, ST0 + 8192 + (i + 1) * 1024) for i in range(8)]
        T_vs = [P.tok("vs%d" % i, ST0 + 16384 + i * 1024, ST0 + 16384 + (i + 1) * 1024) for i in range(8)]
        AT0 = ST0 + 24576
        off = [AT0]

        def atemp(nbytes, dt, name):
            o = off[0]
            off[0] += nbytes
            return carve(o, nbytes, dt), P.tok(name, o, o + nbytes)

        qsc = [atemp(2048, F32, "qsc%d" % i) for i in range(2)]
        rt1 = [atemp(2048, F32, "rt1_%d" % i) for i in range(2)]
        rt2 = [atemp(2048, F32, "rt2_%d" % i) for i in range(2)]
        tanh_t = [atemp(2048, F32, "tanh%d" % i) for i in range(2)]
        qkT = [atemp(2048, BF16, "qkT%d" % i) for i in range(2)]
        sTb = [atemp(1024, BF16, "sT%d" % i) for i in range(2)]
        yrb = [atemp(1024, BF16, "yr%d" % i) for i in range(2)]
        sgb = [atemp(1024, BF16, "sg%d" % i) for i in range(2)]
        assert off[0] <= UBYTES, off[0]
        DGB = 31 * 256
        diag = [carve(AT0 + j * DGB, DGB, BF16).rearrange("p (k n) -> p k n", k=31) for j in range(2)]
        T_diag = [P.tok("diag%d" % j, AT0 + j * DGB, AT0 + (j + 1) * DGB) for j in range(2)]
        assert 2 * DGB <= 16384

        banks = [es.enter_context(nc.psum_tensor("ps%d" % i, [128, 512], F32)) for i in range(8)]
        T_bank = [P.tok("bank%d" % i) for i in range(8)]
        bptr = [0]

        def bank():
            i = bptr[0] % 8
            bptr[0] += 1
            return banks[i], T_bank[i]

        rptr = {"s": 0, "o": 0}

        def bank_short():
            i = rptr["s"] % 4
            rptr["s"] += 1
            return banks[i], T_bank[i]

        def bank_o():
            i = 4 + rptr["o"] % 2
            rptr["o"] += 1
            return banks[i], T_bank[i]

        smptr = [0]

        def small():
            i = smptr[0] % NSM
            smptr[0] += 1
            return sm[i], T_sm[i]

        cnt = {"xn": 0, "tmp": 0}

        def ncd(fn):
            def f(e):
                with nc.allow_non_contiguous_dma(reason="small strided constant load"):
                    return fn(e)
            return f

        dumped = set()
        cur_g = [0]

        def dump(name, ap, toks):
            if name not in dbg or name in dumped or cur_g[0] != dbg_group:
                return
            dumped.add(name)
            d = nc.dram_tensor("dbg_" + name, list(ap.shape), F32, kind="ExternalOutput").ap()
            P.op("pool", lambda e: e.dma_start(out=d, in_=ap), reads=toks, stream="dbg_" + name)

        def stage(name):
            if stop == name:
                raise _Stop()

        W = [T_const]
        P.op("sp", lambda e: e.dma_start(out=identf[:], in_=c_ident[:, :]), writes=W, stream="c")
        P.op("sp", lambda e: e.dma_start(out=maskb[:], in_=c_mask[:, :]), writes=W, stream="c")
        P.op("sp", lambda e: e.dma_start(out=decb[:], in_=c_dec[:, :]), writes=W, stream="c")
        P.op("sp", ncd(lambda e: e.dma_start(out=gpre[:], in_=g_pre.rearrange("(k p) -> p k", p=128))), writes=W, stream="c")
        P.op("sp", ncd(lambda e: e.dma_start(out=gfpre[:], in_=g_fpre.rearrange("(k p) -> p k", p=128))), writes=W, stream="c")
        P.op("sp", ncd(lambda e: e.dma_start(out=cb[:], in_=dw_b.rearrange("(c p) -> p c", p=128))), writes=W, stream="c")
        P.op("sp", ncd(lambda e: e.dma_start(out=lnw[:], in_=ln_w.rearrange("(c p) -> p c", p=128))), writes=W, stream="c")
        P.op("sp", ncd(lambda e: e.dma_start(out=lnb[:], in_=ln_b.rearrange("(c p) -> p c", p=128))), writes=W, stream="c")
        for c in range(4):
            P.op("sp", ncd(lambda e, c=c: e.dma_start(out=cw[:, c, :], in_=dw_w[:, c * 128:(c + 1) * 128].rearrange("k p -> p k"))),
                 writes=W, stream="c")
        T_c2 = P.tok("const2")
        P.op("dve", lambda e: e.tensor_copy(out=identb[:], in_=identf[:]), reads=W, writes=[T_c2])
        P.op("dve", lambda e: e.memset(ones_s[:], 1.0 / 512), writes=[T_c2])
        P.op("dve", lambda e: e.memset(nhalf[:], -0.5), writes=[T_c2])
        P.op("dve", lambda e: e.memset(epsb[:, 0:4], EPS), writes=[T_c2])
        P.op("dve", lambda e: e.memset(epsb[:, 4:8], 4 * EPS), writes=[T_c2])
        P.op("dve", lambda e: e.tensor_scalar(out=cw[:], in0=cw[:], scalar1=0.5, scalar2=None, op0=ALU.mult),
             reads=W, writes=[T_c2])
        CONST = [T_const, T_c2]

        def rstd_chain(ss_ap, ss_tok, n, scale, width=1, eps4=False):
            v, tv = small()
            eo = 4 if eps4 else 0
            P.op("dve", lambda e: e.scalar_tensor_tensor(out=v[:, 0:width], in0=ss_ap, scalar=scale, in1=epsb[:, eo:eo + width],
                                                         op0=ALU.mult, op1=ALU.add), reads=[ss_tok] + CONST, writes=[tv])
            r, tr = small()
            P.op("pool", lambda e: e.tensor_tensor(out=r[:, 0:width], in0=v[:, 0:width], in1=nhalf[:, 0:width], op=ALU.pow),
                 reads=[tv] + CONST, writes=[tr])
            return r, tr

        def norm_phase(gain_ap):
            rs_ = {}
            evs = {}
            for i in range(11):
                if i < 8:
                    s_, ts = small()
                    P.op("act", lambda e, i=i, s_=s_: e.activation(out=junk[:], in_=xt[:, i, :], func=AF.Square, accum_out=s_[:, 0:1]),
                         reads=[T_xt[i]], writes=[T_junk, ts])
                    v, tv = small()
                    P.op("dve", lambda e, s_=s_, v=v: e.scalar_tensor_tensor(out=v[:, 0:1], in0=s_[:, 0:1], scalar=1.0 / 1024, in1=epsb[:, 0:1],
                                                                             op0=ALU.mult, op1=ALU.add), reads=[ts] + CONST, writes=[tv])
                    r, tr = small()
                    P.op("pool", lambda e, v=v, r=r: e.tensor_tensor(out=r[:, 0:1], in0=v[:, 0:1], in1=nhalf[:, 0:1], op=ALU.pow),
                         reads=[tv] + CONST, writes=[tr])
                    rs_[i] = (r, tr)
                j = i - 2
                if 0 <= j < 8:
                    r, tr = rs_[j]
                    b = cnt["xn"] % 2
                    cnt["xn"] += 1
                    P.op("act", lambda e, j=j, b=b, r=r: e.activation(out=xn[b][:], in_=xt[:, j, :], func=AF.Copy, scale=r[:, 0:1]),
                         reads=[T_xt[j], tr], writes=[T_xn[b]])
                    evs[j] = transposes_to_hT(j, b, gain_ap)
                j = i - 3
                if 0 <= j < 8:
                    evs[j]()

        def transposes_to_hT(i, b, gain_ap):
            ps, tps = bank()
            psb = ps[:].bitcast(BF16)

            def tr(e):
                for k in range(8):
                    ins = e.transpose(out=psb[:, k * 128:(k + 1) * 128], in_=xn[b][:, k * 128:(k + 1) * 128], identity=identb[:])
                return ins
            P.op("pe", tr, reads=[T_xn[b]] + CONST, writes=[tps])
            dst = hT[:, :, i * 128:(i + 1) * 128]
            src = psb.rearrange("p (k t) -> p k t", k=8)

            def evac():
                if gain_ap is not None:
                    P.op("dve", lambda e: e.tensor_tensor(out=dst, in0=src, in1=gain_ap.unsqueeze(2).to_broadcast([128, 8, 128]),
                                                           op=ALU.mult), reads=[tps] + CONST, writes=[T_hT[i]])
                else:
                    P.op("act", lambda e: e.activation(out=dst, in_=src, func=AF.Copy), reads=[tps], writes=[T_hT[i]])
            return evac

        def load_gbc(vec):
            P.op("sp", lambda e: e.dma_start(out=gbc[:], in_=vec.partition_broadcast(128)), writes=[T_gbc], stream="g")

        def post_norm_residual(i, pss, in_is_psum, scale, out_ap=None, out_tok=None, extra_rscale=None):
            s, ts = small()
            for nh_ in range(2):
                src, tsrc = pss[nh_]
                P.op("act", lambda e, src=src, nh_=nh_: e.activation(out=junk[:, 0:512], in_=src, func=AF.Square,
                                                                    accum_out=s[:, nh_:nh_ + 1]),
                     reads=[tsrc], writes=[T_junk, ts])
            s2, ts2 = small()
            P.op("dve", lambda e: e.tensor_tensor(out=s2[:, 0:1], in0=s[:, 0:1], in1=s[:, 1:2], op=ALU.add), reads=[ts], writes=[ts2])
            if extra_rscale is not None:
                assert extra_rscale == 0.5
                r, tr = rstd_chain(s2[:, 0:1], ts2, 1, 4.0 * scale, eps4=True)
            else:
                r, tr = rstd_chain(s2[:, 0:1], ts2, 1, scale)
            for nh_ in range(2):
                src, tsrc = pss[nh_]
                tb = cnt["tmp"] % 4
                cnt["tmp"] += 1
                P.op("dve", lambda e, src=src, tb=tb, nh_=nh_, r=r: e.scalar_tensor_tensor(
                    out=tmp32[tb][:], in0=src, scalar=r[:, 0:1], in1=gbc[:, nh_ * 512:(nh_ + 1) * 512], op0=ALU.mult, op1=ALU.mult),
                    reads=[tsrc, tr, T_gbc], writes=[T_tmp32[tb]])
                if out_ap is None:
                    dst = xt[:, i, nh_ * 512:(nh_ + 1) * 512]
                    P.op("dve", lambda e, dst=dst, tb=tb: e.tensor_tensor(out=dst, in0=dst, in1=tmp32[tb][:], op=ALU.add),
                         reads=[T_tmp32[tb], T_xt[i]], writes=[T_xt[i]])
                else:
                    dst = out_ap[:, nh_ * 512:(nh_ + 1) * 512]
                    P.op("dve", lambda e, dst=dst, tb=tb, nh_=nh_: e.tensor_tensor(
                        out=dst, in0=xt[:, i, nh_ * 512:(nh_ + 1) * 512], in1=tmp32[tb][:], op=ALU.add),
                        reads=[T_tmp32[tb], T_xt[i]], writes=[out_tok])

        w_in_v = w_in.rearrange("(k p) n -> p k n", p=128)
        w_out_v = w_out.rearrange("(k p) n -> p k n", p=128)
        w_gate_v = w_gate.rearrange("(k p) n -> p k n", p=128)
        w_up_v = w_up.rearrange("(k p) n -> p k n", p=128)
        w_down_v = w_down.rearrange("(c p) n -> p c n", p=128)
        w_pg_v = w_pg.rearrange("(k p) n -> p k n", p=128)
        w_pp_v = w_pp.rearrange("(k p) n -> p k n", p=128)

        def load_ring(slot, cc):
            P.op("pool", lambda e: e.dma_start(out=ring[slot][:], in_=w_in_v[:, :, cc * 512:(cc + 1) * 512]),
                 writes=[T_ring[slot]], stream="ring%d" % slot)

        try:
          for g in range(ngroups):
              cur_g[0] = g
              half = g % 2
              tok0 = g * GT
              pos0 = half * GT

              P.op("sp", lambda e, pos0=pos0: e.dma_start(out=cosb[:], in_=c_cos[pos0:pos0 + GT, :].rearrange("(i p) d -> p i d", p=128)),
                   writes=[T_cos], stream="tab0")
              P.op("sp", lambda e, pos0=pos0: e.dma_start(out=sinb[:], in_=c_sin[pos0:pos0 + GT, :].rearrange("(i p) d -> p i d", p=128)),
                   writes=[T_sin], stream="tab1")
              load_ring(0, 0)
              load_ring(1, 1)
              if half == 0:
                  P.op("pool", lambda e: e.memset(S32[:], 0.0), writes=[T_S32])
                  P.op("pool", lambda e: e.memset(Sbf[:], 0.0), writes=[T_Sbf])
                  for c in range(4):
                      P.op("pool", lambda e, c=c: e.memset(glu[:, c, 0:30], 0.0), writes=[T_glu[c][0]])
              else:
                  for c in range(4):
                      P.op("pool", lambda e, c=c: e.tensor_copy(out=glu[:, c, 0:30], in_=halo_sv[:, c, :]),
                           reads=[T_halo], writes=[T_glu[c][0]])

              for i in range(8):
                  P.op("sp", lambda e, i=i, tok0=tok0: e.dma_start(out=xt[:, i, :], in_=x[tok0 + i * 128: tok0 + (i + 1) * 128, :]),
                       writes=[T_xt[i]], stream="x%d" % i)
              norm_phase(gpre[:])

              dump('hT', hT[:], T_hT)
              stage('p1')
              for c in range(4):
                  for th in range(2):
                      psA, tA = bank()
                      psB, tB = bank()

                      def mmab(e, c=c, th=th, psA=psA, psB=psB):
                          for k in range(8):
                              e.matmul(psA[:], lhsT=ring[0][:, k, c * 128:(c + 1) * 128], rhs=hT[:, k, th * 512:(th + 1) * 512],
                                       start=(k == 0), stop=(k == 7))
                          for k in range(8):
                              ins = e.matmul(psB[:], lhsT=ring[1][:, k, c * 128:(c + 1) * 128], rhs=hT[:, k, th * 512:(th + 1) * 512],
                                             start=(k == 0), stop=(k == 7))
                          return ins
                      P.op("pe", mmab, reads=T_ring + T_hT[th * 4:(th + 1) * 4], writes=[tA, tB])
                      tb = (c * 2 + th) % 2
                      th_ap, th_tok = tanh_t[tb]
                      P.op("act", lambda e, psB=psB, th_ap=th_ap: e.activation(out=th_ap, in_=psB[:], func=AF.Tanh, scale=0.5),
                           reads=[tB], writes=[th_tok])
                      P.op("dve", lambda e, c=c, th=th, psA=psA, th_ap=th_ap: e.scalar_tensor_tensor(
                          out=glu[:, c, 30 + th * 512: 30 + (th + 1) * 512], in0=th_ap, scalar=1.0, in1=psA[:],
                          op0=ALU.add, op1=ALU.mult), reads=[tA, th_tok], writes=[T_glu[c][1 + th]])

              for c in range(4):
                  P.op("pool", lambda e, c=c: e.tensor_copy(out=halo_sv[:, c, :], in_=glu[:, c, GT:GT + 30]),
                       reads=[T_glu[c][2]], writes=[T_halo])
              dump('glu', glu, [t for tt in T_glu for t in tt])
              stage('glu')
              load_ring(0, 2)
              load_ring(1, 3)
              for which in range(3):
                  slot = which % 2
                  if which == 2:
                      load_ring(0, 4)
                  for i in range(8):
                      ps, tps = bank()

                      def mmq(e, i=i, ps=ps, slot=slot):
                          for k in range(8):
                              ins = e.matmul(ps[:], lhsT=hT[:, k, i * 128:(i + 1) * 128], rhs=ring[slot][:, k, :],
                                             start=(k == 0), stop=(k == 7))
                          return ins
                      P.op("pe", mmq, reads=[T_ring[slot], T_hT[i]], writes=[tps])
                      if which == 2:
                          P.op("act", lambda e, i=i, ps=ps: e.activation(out=vs[:, i, :], in_=ps[:], func=AF.Copy),
                               reads=[tps], writes=[T_vs[i]])
                          continue
                      tb = i % 2
                      q_ap, q_tok = qsc[tb]
                      for h in range(4):
                          P.op("act", lambda e, h=h, ps=ps, q_ap=q_ap, which=which: e.activation(
                              out=q_ap[:, h * 128:(h + 1) * 128], in_=ps[:, h * 128:(h + 1) * 128], func=AF.Copy,
                              scale=decb[:, which * 4 + h: which * 4 + h + 1]), reads=[tps] + CONST, writes=[q_tok])
                      t1_ap, t1_tok = rt1[tb]
                      t2_ap, t2_tok = rt2[tb]
                      q3 = q_ap.rearrange("p (h d) -> p h d", h=4)
                      t13 = t1_ap.rearrange("p (h d) -> p h d", h=4)
                      t23 = t2_ap.rearrange("p (h d) -> p h d", h=4)
                      P.op("dve", lambda e, i=i, q3=q3, t13=t13: e.tensor_tensor(
                          out=t13, in0=q3, in1=cosb[:, i, :].unsqueeze(1).to_broadcast([128, 4, 128]), op=ALU.mult),
                          reads=[q_tok, T_cos], writes=[t1_tok])
                      P.op("pool", lambda e, i=i, q3=q3, t23=t23: e.tensor_tensor(
                          out=t23[:, :, 0:64], in0=q3[:, :, 64:128], in1=sinb[:, i, 0:64].unsqueeze(1).to_broadcast([128, 4, 64]),
                          op=ALU.mult), reads=[q_tok, T_sin], writes=[t2_tok])
                      P.op("pool", lambda e, i=i, q3=q3, t23=t23: e.tensor_tensor(
                          out=t23[:, :, 64:128], in0=q3[:, :, 0:64], in1=sinb[:, i, 64:128].unsqueeze(1).to_broadcast([128, 4, 64]),
                          op=ALU.mult), reads=[q_tok, T_sin], writes=[t2_tok])
                      dst = (qs if which == 0 else ks)[:, i, :]
                      dtok = (T_qs if which == 0 else T_ks)[i]
                      P.op("dve", lambda e, dst=dst, t1_ap=t1_ap, t2_ap=t2_ap: e.tensor_tensor(out=dst, in0=t1_ap, in1=t2_ap, op=ALU.add),
                           reads=[t1_tok, t2_tok], writes=[dtok])
              load_ring(1, 5)

              dump('qs', qs, T_qs)
              dump('ks', ks, T_ks)
              dump('vs', vs, T_vs)
              stage('qkv')
              conv_blocks = [(th, c) for th in range(2) for c in range(4)]

              def diag_build(bi):
                  th, c = conv_blocks[bi]
                  j = bi % 2
                  P.op("dve", lambda e, c=c, j=j: e.tensor_tensor(out=diag[j], in0=identf[:].unsqueeze(1).to_broadcast([128, 31, 128]),
                                                                 in1=cw[:, c, :].unsqueeze(2).to_broadcast([128, 31, 128]), op=ALU.mult),
                       reads=CONST, writes=[T_diag[j]])

              def conv_part(bi, part):
                  th, c = conv_blocks[bi]
                  j = bi % 2
                  cps, tcps = banks[6 + j], T_bank[6 + j]
                  k0, k1 = [(0, 10), (10, 20), (20, 31)][part]
                  rtoks = [T_glu[c][1 + th]] + ([T_glu[c][0]] if th == 0 else [T_glu[c][1]])

                  def mmc(e):
                      for k in range(k0, k1):
                          ins = e.matmul(cps[:], lhsT=diag[j][:, k, :], rhs=glu[:, c, th * 512 + k: th * 512 + k + 512],
                                         start=(k == 0), stop=(k == 30))
                      return ins
                  P.op("pe", mmc, reads=rtoks + [T_diag[j]], writes=[tcps])
                  if part == 2:
                      P.op("act", lambda e: e.activation(out=acc[c], in_=cps[:], func=AF.Identity, bias=cb[:, c:c + 1]),
                           reads=[tcps] + CONST, writes=[T_acc[c]])
                      P.op("act", lambda e: e.activation(out=ybf[:, c, :], in_=cps[:], func=AF.Identity, bias=cb[:, c:c + 1]),
                           reads=[tcps] + CONST, writes=[T_ybf[c]])
                      P.op("act", lambda e: e.activation(out=ysq[:, c, :], in_=cps[:], func=AF.Square, bias=cb[:, c:c + 1]),
                           reads=[tcps] + CONST, writes=[T_ysq[c]])
                      if c == 3:
                          conv_finish(th)
                      if bi + 1 < len(conv_blocks):
                          diag_build(bi + 1)

              def conv_finish(th):
                  psM, tM = bank_short()
                  psQ, tQ = bank_short()

                  def mmst(e):
                      for c in range(4):
                          e.matmul(psM[:], lhsT=ones_s[:], rhs=ybf[:, c, :], start=(c == 0), stop=(c == 3))
                      for c in range(4):
                          ins = e.matmul(psQ[:], lhsT=ones_s[:], rhs=ysq[:, c, :], start=(c == 0), stop=(c == 3))
                      return ins
                  P.op("pe", mmst, reads=T_ybf + T_ysq + CONST, writes=[tM, tQ])
                  P.op("act", lambda e: e.activation(out=mu_sb, in_=psM[:], func=AF.Copy), reads=[tM], writes=[T_mu])
                  P.op("dve", lambda e: e.tensor_tensor(out=musq, in0=mu_sb, in1=mu_sb, op=ALU.mult), reads=[T_mu], writes=[T_musq])
                  P.op("dve", lambda e: e.scalar_tensor_tensor(out=musq, in0=psQ[:], scalar=EPS, in1=musq, op0=ALU.add, op1=ALU.subtract),
                       reads=[tQ, T_musq], writes=[T_musq])
                  P.op("act", lambda e: e.activation(out=rstdb, in_=musq, func=AF.Sqrt), reads=[T_musq], writes=[T_rstdb])
                  P.op("dve", lambda e: e.reciprocal(out=rstdb, in_=rstdb), reads=[T_rstdb], writes=[T_rstdb])
                  for c in range(4):
                      P.op("dve", lambda e, c=c: e.tensor_tensor(out=acc[c], in0=acc[c], in1=mu_sb, op=ALU.subtract),
                           reads=[T_acc[c], T_mu], writes=[T_acc[c]])
                  for c in range(4):
                      P.op("dve", lambda e, c=c: e.tensor_tensor(out=acc[c], in0=acc[c], in1=rstdb, op=ALU.mult),
                           reads=[T_acc[c], T_rstdb], writes=[T_acc[c]])
                  for c in range(4):
                      P.op("act", lambda e, c=c: e.activation(out=catT[:, c, th * 512:(th + 1) * 512], in_=acc[c], func=AF.Silu,
                                                              scale=lnw[:, c:c + 1], bias=lnb[:, c:c + 1]),
                           reads=[T_acc[c]] + CONST, writes=T_cat[c][th * 4:(th + 1) * 4])

              ctx = {}

              def ret_A(i):
                  tb = i % 2
                  psg, tpsg = bank_short()

                  def mmg(e):
                      for k in range(8):
                          ins = e.matmul(psg[:], lhsT=hT[:, k, i * 128:(i + 1) * 128], rhs=ring[1][:, k, :], start=(k == 0), stop=(k == 7))
                      return ins
                  P.op("pe", mmg, reads=[T_ring[1], T_hT[i]], writes=[tpsg])
                  sg_ap, sg_tok = sgb[tb]
                  P.op("act", lambda e: e.activation(out=sg_ap, in_=psg[:], func=AF.Silu), reads=[tpsg], writes=[sg_tok])
                  pst, tpst = bank_short()
                  pstb = pst[:].bitcast(BF16)

                  def trqk(e):
                      for h in range(4):
                          e.transpose(out=pstb[:, h * 128:(h + 1) * 128], in_=qs[:, i, h * 128:(h + 1) * 128], identity=identb[:])
                      for h in range(4):
                          ins = e.transpose(out=pstb[:, 512 + h * 128: 512 + (h + 1) * 128], in_=ks[:, i, h * 128:(h + 1) * 128], identity=identb[:])
                      return ins
                  P.op("pe", trqk, reads=[T_qs[i], T_ks[i]] + CONST, writes=[tpst])
                  qk_ap, qk_tok = qkT[tb]
                  P.op("dve", lambda e: e.tensor_copy(out=qk_ap, in_=pstb), reads=[tpst], writes=[qk_tok])
                  ctx[i] = dict(sg=(sg_ap, sg_tok), qk=(qk_ap, qk_tok))

              def ret_A2(i):
                  tb = i % 2
                  qk_ap, qk_tok = ctx[i]["qk"]
                  pss, tpss = bank_short()

                  def mms(e):
                      for h in range(4):
                          ins = e.matmul(pss[:, h * 128:(h + 1) * 128], lhsT=qk_ap[:, 512 + h * 128: 512 + (h + 1) * 128],
                                         rhs=qk_ap[:, h * 128:(h + 1) * 128], start=True, stop=True)
                      return ins
                  P.op("pe", mms, reads=[qk_tok], writes=[tpss])
                  sT_ap, sT_tok = sTb[tb]
                  P.op("dve", lambda e: e.tensor_tensor(out=sT_ap, in0=pss[:], in1=maskb[:], op=ALU.mult),
                       reads=[tpss] + CONST, writes=[sT_tok])
                  ctx[i]["sT"] = (sT_ap, sT_tok)

              def ret_B1(i):
                  qk_ap, qk_tok = ctx[i]["qk"]
                  sT_ap, sT_tok = ctx[i]["sT"]
                  pso, tpso = bank_o()

                  def mmo(e):
                      for h in range(4):
                          e.matmul(pso[:, h * 128:(h + 1) * 128], lhsT=sT_ap[:, h * 128:(h + 1) * 128], rhs=vs[:, i, h * 128:(h + 1) * 128],
                                   start=True, stop=False)
                          ins = e.matmul(pso[:, h * 128:(h + 1) * 128], lhsT=qk_ap[:, h * 128:(h + 1) * 128], rhs=Sbf[:, h * 128:(h + 1) * 128],
                                         start=False, stop=True)
                      return ins
                  P.op("pe", mmo, reads=[sT_tok, T_vs[i], qk_tok, T_Sbf], writes=[tpso])
                  pkv, tpkv = bank_short()

                  def mmkv(e):
                      for h in range(4):
                          ins = e.matmul(pkv[:, h * 128:(h + 1) * 128], lhsT=ks[:, i, h * 128:(h + 1) * 128], rhs=vs[:, i, h * 128:(h + 1) * 128],
                                         start=True, stop=True)
                      return ins
                  P.op("pe", mmkv, reads=[T_ks[i], T_vs[i]], writes=[tpkv])
                  for h in range(4):
                      P.op("dve", lambda e, h=h: e.scalar_tensor_tensor(
                          out=S32[:, h * 128:(h + 1) * 128], in0=S32[:, h * 128:(h + 1) * 128], scalar=g128[h],
                          in1=pkv[:, h * 128:(h + 1) * 128], op0=ALU.mult, op1=ALU.add), reads=[tpkv, T_S32], writes=[T_S32])
                  P.op("act", lambda e: e.activation(out=Sbf[:], in_=S32[:], func=AF.Copy), reads=[T_S32], writes=[T_Sbf])
                  so, tso = small()
                  for h in range(4):
                      P.op("act", lambda e, h=h: e.activation(out=junk[:, 0:128], in_=pso[:, h * 128:(h + 1) * 128],
                                                              func=AF.Square, accum_out=so[:, h:h + 1]),
                           reads=[tpso], writes=[T_junk, tso])
                  r4, tr4 = rstd_chain(so[:, 0:4], tso, 4, 1.0 / 128, width=4)
                  ctx[i]["o"] = (pso, tpso, r4, tr4)

              def ret_B2(i):
                  tb = i % 2
                  pso, tpso, r4, tr4 = ctx[i]["o"]
                  sg_ap, sg_tok = ctx[i]["sg"]
                  yr_ap, yr_tok = yrb[tb]
                  for h in range(4):
                      P.op("dve", lambda e, h=h: e.scalar_tensor_tensor(
                          out=yr_ap[:, h * 128:(h + 1) * 128], in0=pso[:, h * 128:(h + 1) * 128], scalar=r4[:, h:h + 1],
                          in1=sg_ap[:, h * 128:(h + 1) * 128], op0=ALU.mult, op1=ALU.mult), reads=[tpso, tr4, sg_tok], writes=[yr_tok])
                  ctx[i]["yr"] = (yr_ap, yr_tok)

              def ret_B3(i):
                  yr_ap, yr_tok = ctx[i]["yr"]
                  psy, tpsy = bank_short()
                  psyb = psy[:].bitcast(BF16)

                  def try_(e):
                      for h in range(4):
                          ins = e.transpose(out=psyb[:, h * 128:(h + 1) * 128], in_=yr_ap[:, h * 128:(h + 1) * 128], identity=identb[:])
                      return ins
                  P.op("pe", try_, reads=[yr_tok] + CONST, writes=[tpsy])
                  P.op("act", lambda e: e.activation(out=catT[:, 4:8, i * 128:(i + 1) * 128],
                                                     in_=psyb[:, 0:512].rearrange("p (h t) -> p h t", h=4), func=AF.Copy),
                       reads=[tpsy], writes=[T_cat[4 + h][i] for h in range(4)])

              diag_build(0)
              ret_A(0)
              ret_A2(0)
              for i in range(8):
                  conv_part(i, 0)
                  if i + 1 < 8:
                      ret_A(i + 1)
                  ret_B1(i)
                  conv_part(i, 1)
                  if i + 1 < 8:
                      ret_A2(i + 1)
                  ret_B2(i)
                  if i >= 1:
                      ret_B3(i - 1)
                  conv_part(i, 2)
              ret_B3(7)

              dump('catT', catT, [t for tt in T_cat for t in tt])
              stage('cat')
              P.op("pool", lambda e: e.dma_start(out=wo[:], in_=w_out_v), writes=[T_wo], stream="wo")
              load_gbc(g_post)
              for i in range(8):
                  pss2 = []
                  for nh_ in range(2):
                      ps, tps = bank()

                      def mmwo(e, i=i, nh_=nh_, ps=ps):
                          for k in range(8):
                              ins = e.matmul(ps[:], lhsT=catT[:, k, i * 128:(i + 1) * 128], rhs=wo[:, k, nh_ * 512:(nh_ + 1) * 512],
                                             start=(k == 0), stop=(k == 7))
                          return ins
                      P.op("pe", mmwo, reads=[T_wo] + [T_cat[k][i] for k in range(8)], writes=[tps])
                      pss2.append((ps[:], tps))
                  post_norm_residual(i, pss2, True, 1.0 / 1024)

              dump('x1', xt[:], T_xt)
              stage('x1')
              for j in range(2):
                  P.op("pool", lambda e, j=j: e.dma_start(out=wd[:, j * 11:(j + 1) * 11, :], in_=w_down_v[:, j * 11:(j + 1) * 11, :]),
                       writes=[T_wd[j]], stream="wd%d" % j)
              norm_phase(gfpre[:])
              load_gbc(g_fpost)
              for st in range(11):
                  slot = st % 2
                  P.op("pool", lambda e, st=st, slot=slot: e.dma_start(out=wgr[slot][:], in_=w_gate_v[:, :, st * 256:(st + 1) * 256]),
                       writes=[T_wgr[slot]], stream="wg%d" % slot)
                  P.op("pool", lambda e, st=st, slot=slot: e.dma_start(out=wur[slot][:], in_=w_up_v[:, :, st * 256:(st + 1) * 256]),
                       writes=[T_wur[slot]], stream="wu%d" % slot)
                  for sub in range(2):
                      c = st * 2 + sub
                      for th in range(2):
                          psG, tG = bank()
                          psU, tU = bank()

                          def mmgu(e, sub=sub, th=th, slot=slot, psG=psG, psU=psU):
                              for k in range(8):
                                  e.matmul(psG[:], lhsT=wgr[slot][:, k, sub * 128:(sub + 1) * 128], rhs=hT[:, k, th * 512:(th + 1) * 512],
                                           start=(k == 0), stop=(k == 7))
                              for k in range(8):
                                  ins = e.matmul(psU[:], lhsT=wur[slot][:, k, sub * 128:(sub + 1) * 128], rhs=hT[:, k, th * 512:(th + 1) * 512],
                                                 start=(k == 0), stop=(k == 7))
                              return ins
                          P.op("pe", mmgu, reads=[T_wgr[slot], T_wur[slot]] + T_hT[th * 4:(th + 1) * 4], writes=[tG, tU])
                          tb = th
                          P.op("act", lambda e, psG=psG, tb=tb: e.activation(out=sgt[tb], in_=psG[:], func=AF.Silu), reads=[tG], writes=[T_sgt[tb]])
                          P.op("dve", lambda e, c=c, th=th, psU=psU, tb=tb: e.tensor_tensor(
                              out=aT[:, c, th * 512:(th + 1) * 512], in0=psU[:], in1=sgt[tb], op=ALU.mult),
                              reads=[tU, T_sgt[tb]], writes=[T_a[c][th]])
              for i in range(8):
                  pss2 = []
                  for nh_ in range(2):
                      ps, tps = bank()

                      def mmd(e, i=i, nh_=nh_, ps=ps):
                          for c in range(NFC):
                              ins = e.matmul(ps[:], lhsT=aT[:, c, i * 128:(i + 1) * 128], rhs=wd[:, c, nh_ * 512:(nh_ + 1) * 512],
                                             start=(c == 0), stop=(c == NFC - 1))
                          return ins
                      P.op("pe", mmd, reads=T_wd + [T_a[c][i // 4] for c in range(NFC)], writes=[tps])
                      pss2.append((ps[:], tps))
                  post_norm_residual(i, pss2, True, 1.0 / 1024)

              dump('x2', xt[:], T_xt)
              stage('x2')
              def load_p(i):
                  pb = i % 2
                  P.op("sp", lambda e, i=i, pb=pb, tok0=tok0: e.dma_start(out=pt32[pb], in_=p[tok0 + i * 128: tok0 + (i + 1) * 128, :]),
                       writes=[T_pt32[pb]], stream="p%d" % pb)
              load_p(0)
              load_p(1)
              P.op("pool", lambda e: e.dma_start(out=wpg[:], in_=w_pg_v), writes=[T_wpg], stream="wpg")
              P.op("pool", lambda e: e.dma_start(out=wpp[:], in_=w_pp_v), writes=[T_wpp], stream="wpp")
              load_gbc(g_ple)
              pend = None
              for i in range(9):
                  nxt = None
                  if i < 8:
                      b = cnt["xn"] % 2
                      cnt["xn"] += 1
                      P.op("act", lambda e, i=i, b=b: e.activation(out=xn[b][:], in_=xt[:, i, :], func=AF.Copy), reads=[T_xt[i]], writes=[T_xn[b]])
                      ev1 = transposes_to_hT(i, b, None)
                      pb = i % 2
                      P.op("act", lambda e, pb=pb: e.activation(out=ptb[pb], in_=pt32[pb], func=AF.Copy), reads=[T_pt32[pb]], writes=[T_ptb[pb]])
                      if i + 2 < 8:
                          load_p(i + 2)
                      psp, tpsp = bank()
                      pspb = psp[:].bitcast(BF16)

                      def trp(e, pb=pb, pspb=pspb):
                          for k in range(2):
                              ins = e.transpose(out=pspb[:, k * 128:(k + 1) * 128], in_=ptb[pb][:, k * 128:(k + 1) * 128], identity=identb[:])
                          return ins
                      P.op("pe", trp, reads=[T_ptb[pb]] + CONST, writes=[tpsp])

                      def nxt(i=i, pspb=pspb, tpsp=tpsp, ev1=ev1):
                          ev1()
                          P.op("act", lambda e: e.activation(out=pT[:, :, i * 128:(i + 1) * 128],
                                                             in_=pspb[:, 0:256].rearrange("p (k t) -> p k t", k=2), func=AF.Copy),
                               reads=[tpsp], writes=T_pT[i])
                  if pend is not None:
                      pend()
                  pend = nxt
              for i in range(8):
                  eb = i % 2
                  for nh_ in range(2):
                      psg2, tg2 = bank()
                      psp2, tp2 = bank()

                      def mmple(e, i=i, nh_=nh_, psg2=psg2, psp2=psp2):
                          for k in range(8):
                              e.matmul(psg2[:], lhsT=hT[:, k, i * 128:(i + 1) * 128], rhs=wpg[:, k, nh_ * 512:(nh_ + 1) * 512],
                                       start=(k == 0), stop=(k == 7))
                          for k in range(2):
                              ins = e.matmul(psp2[:], lhsT=pT[:, k, i * 128:(i + 1) * 128], rhs=wpp[:, k, nh_ * 512:(nh_ + 1) * 512],
                                             start=(k == 0), stop=(k == 1))
                          return ins
                      P.op("pe", mmple, reads=[T_hT[i], T_wpg, T_wpp] + T_pT[i], writes=[tg2, tp2])
                      P.op("act", lambda e, nh_=nh_, psg2=psg2, eb=eb: e.activation(out=tt32[eb][:, nh_ * 512:(nh_ + 1) * 512], in_=psg2[:],
                                                                                   func=AF.Tanh, scale=0.5), reads=[tg2], writes=[T_tt32[eb][nh_]])
                      P.op("dve", lambda e, nh_=nh_, psp2=psp2, eb=eb: e.scalar_tensor_tensor(
                          out=e32[eb][:, nh_ * 512:(nh_ + 1) * 512], in0=tt32[eb][:, nh_ * 512:(nh_ + 1) * 512], scalar=1.0, in1=psp2[:],
                          op0=ALU.add, op1=ALU.mult), reads=[tp2, T_tt32[eb][nh_]], writes=[T_e32[eb][nh_]])
                  ob = i % 2
                  post_norm_residual(i, [(e32[eb][:, 0:512], T_e32[eb][0]), (e32[eb][:, 512:1024], T_e32[eb][1])], False, 1.0 / 4096,
                                     out_ap=ot[ob], out_tok=T_ot[ob], extra_rscale=0.5)
                  P.op("sp", lambda e, i=i, ob=ob, tok0=tok0: e.dma_start(out=out[tok0 + i * 128: tok0 + (i + 1) * 128, :], in_=ot[ob]),
                       reads=[T_ot[ob]], stream="out%d" % ob)

              if True:
                  dump('x2T', hT[:], T_hT)
                  dump('pT', pT, [t for tt in T_pT for t in tt])
                  dump('e32', e32[1], T_e32[1])
                  dump('tt32', tt32[1], T_tt32[1])
        except _Stop:
            pass
        P.emit(nc, es)
    return nc


def _constants():
    half = 64
    freqs = 10000.0 ** (-np.arange(half, dtype=np.float64) / half)
    pos = np.arange(2048, dtype=np.float64)
    ang = pos[:, None] * freqs[None, :]
    cos = np.cos(ang).astype(np.float32)
    sin = np.sin(ang).astype(np.float32)
    c_cos = np.concatenate([cos, cos], axis=1)
    c_sin = np.concatenate([-sin, sin], axis=1)
    gam = 1.0 - 2.0 ** (-5.0 - np.arange(4, dtype=np.float64))
    idx = np.arange(128, dtype=np.float64)
    dq = gam[None, :] ** (idx[:, None] + 1.0)
    dk = gam[None, :] ** (127.0 - idx[:, None]) * (128.0 ** -0.5)
    c_dec = np.concatenate([dq, dk], axis=1).astype(np.float32)
    causal = (idx[None, :] >= idx[:, None]).astype(np.float64)
    c_mask = np.stack([causal * gam[h] ** (-128.0) for h in range(4)], axis=1).reshape(128, 512).astype(np.float32)
    return {
        "c_ident": np.eye(128, dtype=np.float32),
        "c_cos": np.ascontiguousarray(c_cos, dtype=np.float32),
        "c_sin": np.ascontiguousarray(c_sin, dtype=np.float32),
        "c_dec": np.ascontiguousarray(c_dec),
        "c_mask": np.ascontiguousarray(c_mask),
    }


def make_in_maps(x, p, mix_pre_norm, w_in, conv_dw_w, conv_dw_b, conv_ln_w, conv_ln_b,
                 w_out, mix_post_norm, ffn_pre_norm, w_ffn_gate, w_ffn_up, w_ffn_down,
                 ffn_post_norm, w_ple_gate, w_ple_proj, ple_post_norm):
    f = lambda a: np.ascontiguousarray(np.asarray(a, dtype=np.float32))
    shared = {
        "w_in": f(w_in[0]), "w_out": f(w_out[0]), "w_gate": f(w_ffn_gate[0]), "w_up": f(w_ffn_up[0]),
        "w_down": f(w_ffn_down[0]), "w_pg": f(w_ple_gate[0]), "w_pp": f(w_ple_proj[0]),
        "g_pre": f(mix_pre_norm[0]), "g_post": f(mix_post_norm[0]), "g_fpre": f(ffn_pre_norm[0]),
        "g_fpost": f(ffn_post_norm[0]), "g_ple": f(ple_post_norm[0]),
        "dw_w": f(conv_dw_w[0]), "dw_b": f(conv_dw_b[0]), "ln_w": f(conv_ln_w[0]), "ln_b": f(conv_ln_b[0]),
    }
    shared.update(_constants())
    xs = f(x).reshape(NCORES, TOK_CORE, 1024)
    ps = f(p[0]).reshape(NCORES, TOK_CORE, 256)
    in_maps = []
    for c in range(NCORES):
        m = dict(shared)
        m["x"] = xs[c]
        m["p"] = ps[c]
        in_maps.append(m)
    return in_maps


def kernel(**inputs):
    in_maps = make_in_maps(**inputs)
    nc = build_program()
    res = run_bass_kernel_spmd(nc, in_maps, core_ids=list(range(NCORES)))
    outs = [np.asarray(r["out"], dtype=np.float32) for r in res.results]
    return np.stack(outs, axis=0).reshape(16, 2048, 1024)
```

```python
import numpy as np
from contextlib import ExitStack
import concourse.bass as bass
import concourse.mybir as mybir
from concourse.bass_utils import run_bass_kernel_spmd

F32 = mybir.dt.float32
BF16 = mybir.dt.bfloat16
ALU = mybir.AluOpType
AF = mybir.ActivationFunctionType

ENGS = ("pe", "act", "dve", "pool", "sp")
EPS = 1e-6
NCORES = 8
TOK_CORE = 4096
GT = 1024
NG = TOK_CORE // GT
DFF = 2816
NFC = DFF // 128


class Tok:
    __slots__ = ("name", "w", "rs", "lo", "hi")

    def __init__(self, name="", lo=None, hi=None):
        self.name = name
        self.w = None
        self.rs = []
        self.lo = lo
        self.hi = hi


class Op:
    __slots__ = ("eng", "fn", "deps", "signal", "semval", "idx", "stream")

    def __init__(self, eng, fn, stream):
        self.eng = eng
        self.fn = fn
        self.stream = stream
        self.deps = []
        self.signal = False
        self.semval = 0
        self.idx = 0


class Prog:
    SAME_ENG_WINDOW = 10 ** 9

    def __init__(self):
        self.q = {e: [] for e in ENGS}
        self.stream_cnt = {}
        self.rtoks = []

    def tok(self, name, lo=None, hi=None):
        t = Tok(name, lo, hi)
        if lo is not None:
            self.rtoks.append(t)
        return t

    def _alias(self, t):
        if t.lo is None:
            return ()
        return [u for u in self.rtoks if u is not t and u.lo < t.hi and t.lo < u.hi]

    def op(self, eng, fn, reads=(), writes=(), stream=None):
        o = Op(eng, fn, stream)
        o.idx = len(self.q[eng])
        deps = {}
        for t in reads:
            if t.w is not None:
                deps[id(t.w)] = t.w
            for u in self._alias(t):
                if u.w is not None:
                    deps[id(u.w)] = u.w
        for t in writes:
            for u in [t] + list(self._alias(t)):
                if u.w is not None:
                    deps[id(u.w)] = u.w
                for r in u.rs:
                    deps[id(r)] = r
        for d in deps.values():
            if d is o:
                continue
            if d.stream is None and d.eng == eng and stream is None:
                if eng == "pe":
                    continue
                if o.idx - d.idx > self.SAME_ENG_WINDOW:
                    continue
            if d.stream is None:
                d.signal = True
            o.deps.append(d)
        for t in reads:
            t.rs.append(o)
        for t in writes:
            t.w = o
            t.rs = []
        if stream is not None:
            c = self.stream_cnt.get(stream, 0) + 1
            self.stream_cnt[stream] = c
            o.semval = 16 * c
        self.q[eng].append(o)
        return o

    def emit(self, nc, es, final_streams=("out",)):
        sems = {e: es.enter_context(nc.semaphore("s_" + e)) for e in ENGS if e != "sp"}
        ssems = {s: es.enter_context(nc.semaphore("d_" + s)) for s in self.stream_cnt}
        for e in ENGS:
            c = 0
            for o in self.q[e]:
                if o.stream is None and o.signal:
                    c += 1
                    o.semval = c
        block = es.enter_context(nc.Block())

        def run(ename, eng):
            waited = {}
            for o in self.q[ename]:
                need = {}
                for d in o.deps:
                    key = ("s", d.stream) if d.stream is not None else ("e", d.eng)
                    if d.semval > need.get(key, 0):
                        need[key] = d.semval
                for key, val in need.items():
                    if waited.get(key, 0) >= val:
                        continue
                    sem = ssems[key[1]] if key[0] == "s" else sems[key[1]]
                    eng.wait_ge(sem, val)
                    waited[key] = val
                ins = o.fn(eng)
                if o.stream is not None:
                    ins.then_inc(ssems[o.stream], 16)
                elif o.signal:
                    ins.then_inc(sems[ename], 1)
            if ename == "sp":
                for s in self.stream_cnt:
                    if s.startswith("out") or s.startswith("dbg"):
                        eng.wait_ge(ssems[s], 16 * self.stream_cnt[s])

        @block.tensor
        def _(eng):
            run("pe", eng)

        @block.scalar
        def _(eng):
            run("act", eng)

        @block.vector
        def _(eng):
            run("dve", eng)

        @block.gpsimd
        def _(eng):
            run("pool", eng)

        @block.sync
        def _(eng):
            run("sp", eng)


class _Stop(Exception):
    pass


def build_program(ngroups=NG, dbg=None, stop=None, dbg_group=0):
    nc = bass.Bass("TRN2", target_bir_lowering=False)
    dbg = dbg or ()

    def din(name, shape):
        return nc.dram_tensor(name, shape, F32, kind="ExternalInput").ap()

    x = din("x", [TOK_CORE, 1024])
    p = din("p", [TOK_CORE, 256])
    w_in = din("w_in", [1024, 3072])
    w_out = din("w_out", [1024, 1024])
    w_gate = din("w_gate", [1024, DFF])
    w_up = din("w_up", [1024, DFF])
    w_down = din("w_down", [DFF, 1024])
    w_pg = din("w_pg", [1024, 1024])
    w_pp = din("w_pp", [256, 1024])
    g_pre = din("g_pre", [1024])
    g_post = din("g_post", [1024])
    g_fpre = din("g_fpre", [1024])
    g_fpost = din("g_fpost", [1024])
    g_ple = din("g_ple", [1024])
    dw_w = din("dw_w", [31, 512])
    dw_b = din("dw_b", [512])
    ln_w = din("ln_w", [512])
    ln_b = din("ln_b", [512])
    c_ident = din("c_ident", [128, 128])
    c_cos = din("c_cos", [2048, 128])
    c_sin = din("c_sin", [2048, 128])
    c_dec = din("c_dec", [128, 8])
    c_mask = din("c_mask", [128, 512])
    out = nc.dram_tensor("out", [TOK_CORE, 1024], F32, kind="ExternalOutput").ap()

    g128 = [float((1.0 - 2.0 ** (-5.0 - h)) ** 128) for h in range(4)]

    es = ExitStack()
    P = Prog()
    with es:
        def sb(name, shape, dt):
            return es.enter_context(nc.sbuf_tensor(name, shape, dt))

        xt = sb("xt", [128, 8, 1024], F32)
        T_xt = [P.tok("xt%d" % i) for i in range(8)]
        hT = sb("hT", [128, 8, GT], BF16)
        T_hT = [P.tok("hT%d" % i) for i in range(8)]
        cosb = sb("cosb", [128, 8, 128], F32)
        sinb = sb("sinb", [128, 8, 128], F32)
        T_cos = P.tok("cos")
        T_sin = P.tok("sin")
        maskb = sb("maskb", [128, 512], F32)
        gbc = sb("gbc", [128, 1024], F32)
        T_gbc = P.tok("gbc")
        identf = sb("identf", [128, 128], F32)
        identb = sb("identb", [128, 128], BF16)
        ones_s = sb("ones_s", [128, 128], BF16)
        nhalf = sb("nhalf", [128, 512], F32)
        epsb = sb("epsb", [128, 8], F32)
        decb = sb("decb", [128, 8], F32)
        gpre = sb("gpre", [128, 8], F32)
        gfpre = sb("gfpre", [128, 8], F32)
        cw = sb("cw", [128, 4, 31], F32)
        cb = sb("cb", [128, 4], F32)
        lnw = sb("lnw", [128, 4], F32)
        lnb = sb("lnb", [128, 4], F32)
        T_const = P.tok("const")
        halo_sv = sb("halo_sv", [128, 4, 30], BF16)
        T_halo = P.tok("halo_sv")
        S32 = sb("S32", [128, 512], F32)
        Sbf = sb("Sbf", [128, 512], BF16)
        T_S32 = P.tok("S32")
        T_Sbf = P.tok("Sbf")
        xn = [sb("xn%d" % i, [128, 1024], BF16) for i in range(2)]
        T_xn = [P.tok("xn%d" % i) for i in range(2)]
        junk = sb("junk", [128, 1024], BF16)
        T_junk = P.tok("junk")
        tmp32 = [sb("tmp32_%d" % i, [128, 512], F32) for i in range(4)]
        T_tmp32 = [P.tok("tmp32_%d" % i) for i in range(4)]
        NSM = 48
        sm = [sb("sm%d" % i, [128, 16], F32) for i in range(NSM)]
        T_sm = [P.tok("sm%d" % i) for i in range(NSM)]

        UBYTES = 120 * 1024
        U = sb("U", [128, UBYTES // 2], BF16)

        def carve(off, nbytes, dt):
            assert off % 4 == 0 and off + nbytes <= UBYTES
            v = U[:, off // 2:(off + nbytes) // 2]
            if dt == F32:
                v = v.bitcast(F32)
            return v

        K = 1024
        aT = carve(0, 45056, BF16).rearrange("p (c t) -> p c t", c=NFC)
        T_a = [[P.tok("a%d_%d" % (c, th), c * 2048 + th * 1024, c * 2048 + th * 1024 + 1024) for th in range(2)]
               for c in range(NFC)]
        wd = carve(45056, 45056, BF16).rearrange("p (c n) -> p c n", c=NFC)
        T_wd = [P.tok("wd%d" % j, 45056 + j * 22528, 45056 + (j + 1) * 22528) for j in range(2)]
        WG0 = 90112
        wgr = [carve(WG0 + i * 4096, 4096, BF16).rearrange("p (k n) -> p k n", k=8) for i in range(2)]
        wur = [carve(WG0 + 8192 + i * 4096, 4096, BF16).rearrange("p (k n) -> p k n", k=8) for i in range(2)]
        T_wgr = [P.tok("wgr%d" % i, WG0 + i * 4096, WG0 + (i + 1) * 4096) for i in range(2)]
        T_wur = [P.tok("wur%d" % i, WG0 + 8192 + i * 4096, WG0 + 8192 + (i + 1) * 4096) for i in range(2)]
        BT0 = WG0 + 16384
        sgt = [carve(BT0 + i * 2048, 2048, F32) for i in range(2)]
        T_sgt = [P.tok("sgt%d" % i, BT0 + i * 2048, BT0 + (i + 1) * 2048) for i in range(2)]
        e32 = [carve(BT0 + 4096, 4096, F32), carve(36864, 4096, F32)]
        T_e32 = [[P.tok("e32_0_%d" % h, BT0 + 4096 + h * 2048, BT0 + 4096 + (h + 1) * 2048) for h in range(2)],
                 [P.tok("e32_1_%d" % h, 36864 + h * 2048, 36864 + (h + 1) * 2048) for h in range(2)]]
        tt32 = [carve(BT0 + 8192, 4096, F32), carve(40960, 4096, F32)]
        T_tt32 = [[P.tok("tt32_0_%d" % h, BT0 + 8192 + h * 2048, BT0 + 8192 + (h + 1) * 2048) for h in range(2)],
                  [P.tok("tt32_1_%d" % h, 40960 + h * 2048, 40960 + (h + 1) * 2048) for h in range(2)]]
        wpg = carve(0, 16384, BF16).rearrange("p (k n) -> p k n", k=8)
        T_wpg = P.tok("wpg", 0, 16384)
        wpp = carve(16384, 4096, BF16).rearrange("p (k n) -> p k n", k=2)
        T_wpp = P.tok("wpp", 16384, 20480)
        pT = carve(20480, 4096, BF16).rearrange("p (k t) -> p k t", k=2)
        T_pT = [[P.tok("pT%d_%d" % (i, k), 20480 + k * 2048 + i * 256, 20480 + k * 2048 + (i + 1) * 256) for k in range(2)]
                for i in range(8)]
        pt32 = [carve(24576 + i * 1024, 1024, F32) for i in range(2)]
        T_pt32 = [P.tok("pt32_%d" % i, 24576 + i * 1024, 24576 + (i + 1) * 1024) for i in range(2)]
        ptb = [carve(26624 + i * 512, 512, BF16) for i in range(2)]
        T_ptb = [P.tok("ptb%d" % i, 26624 + i * 512, 26624 + (i + 1) * 512) for i in range(2)]
        ot = [carve(28672 + i * 4096, 4096, F32) for i in range(2)]
        T_ot = [P.tok("ot%d" % i, 28672 + i * 4096, 28672 + (i + 1) * 4096) for i in range(2)]
        catT = carve(0, 16384, BF16).rearrange("p (k t) -> p k t", k=8)
        T_cat = [[P.tok("cat%d_%d" % (k, j), k * 2048 + j * 256, k * 2048 + (j + 1) * 256) for j in range(8)]
                 for k in range(8)]
        GL0 = 16384
        GLW = 30 + GT
        glu = carve(GL0, 4 * GLW * 2 + 16, BF16)[:, 0:4 * GLW].rearrange("p (c t) -> p c t", c=4)
        T_glu = [[P.tok("glu%d_%d" % (c, j), GL0 + c * GLW * 2 + (0 if j == 0 else 60 + (j - 1) * 1024),
                        GL0 + c * GLW * 2 + (60 if j == 0 else 60 + j * 1024)) for j in range(3)] for c in range(4)]
        YB0 = GL0 + 8448
        ybf = carve(YB0, 4096, BF16).rearrange("p (c t) -> p c t", c=4)
        ysq = carve(YB0 + 4096, 4096, BF16).rearrange("p (c t) -> p c t", c=4)
        T_ybf = [P.tok("ybf%d" % c, YB0 + c * 1024, YB0 + (c + 1) * 1024) for c in range(4)]
        T_ysq = [P.tok("ysq%d" % c, YB0 + 4096 + c * 1024, YB0 + 4096 + (c + 1) * 1024) for c in range(4)]
        AC0 = YB0 + 8192
        acc = [carve(AC0 + c * 2048, 2048, F32) for c in range(4)]
        T_acc = [P.tok("acc%d" % c, AC0 + c * 2048, AC0 + (c + 1) * 2048) for c in range(4)]
        LN0 = AC0 + 8192
        mu_sb = carve(LN0, 2048, F32)
        musq = carve(LN0 + 2048, 2048, F32)
        rstdb = carve(LN0 + 4096, 2048, F32)
        T_mu = P.tok("mu", LN0, LN0 + 2048)
        T_musq = P.tok("musq", LN0 + 2048, LN0 + 4096)
        T_rstdb = P.tok("rstdb", LN0 + 4096, LN0 + 6144)
        assert LN0 + 6144 <= 45056 + 4096
        RG0 = 49152
        ring = [carve(RG0 + i * 8192, 8192, BF16).rearrange("p (k n) -> p k n", k=8) for i in range(2)]
        T_ring = [P.tok("ring%d" % i, RG0 + i * 8192, RG0 + (i + 1) * 8192) for i in range(2)]
        wo = carve(RG0, 16384, BF16).rearrange("p (k n) -> p k n", k=8)
        T_wo = P.tok("wo", RG0, RG0 + 16384)
        ST0 = RG0 + 16384
        qs = carve(ST0, 8192, BF16).rearrange("p (i n) -> p i n", i=8)
        ks = carve(ST0 + 8192, 8192, BF16).rearrange("p (i n) -> p i n", i=8)
        vs = carve(ST0 + 16384, 8192, BF16).rearrange("p (i n) -> p i n", i=8)
        T_qs = [P.tok("qs%d" % i, ST0 + i * 1024, ST0 + (i + 1) * 1024) for i in range(8)]
        T_ks = [P.tok("ks%d" % i, ST0 + 8192 + i * 1024, ST0 + 8192 + (i + 1) * 1024) for i in range(8)]
        T_vs = [P.tok("vs%d" % i, ST0 + 16384 + i * 1024, ST0 + 16384 + (i + 1) * 1024) for i in range(8)]
        AT0 = ST0 + 24576
        off = [AT0]

        def atemp(nbytes, dt, name):
            o = off[0]
            off[0] += nbytes
            return carve(o, nbytes, dt), P.tok(name, o, o + nbytes)

        qsc = [atemp(2048, F32, "qsc%d" % i) for i in range(2)]
        rt1 = [atemp(2048, F32, "rt1_%d" % i) for i in range(2)]
        rt2 = [atemp(2048, F32, "rt2_%d" % i) for i in range(2)]
        tanh_t = [atemp(2048, F32, "tanh%d" % i) for i in range(2)]
        qkT = [atemp(2048, BF16, "qkT%d" % i) for i in range(2)]
        sTb = [atemp(1024, BF16, "sT%d" % i) for i in range(2)]
        yrb = [atemp(1024, BF16, "yr%d" % i) for i in range(2)]
        sgb = [atemp(1024, BF16, "sg%d" % i) for i in range(2)]
        assert off[0] <= UBYTES, off[0]
        DGB = 31 * 256
        diag = [carve(AT0 + j * DGB, DGB, BF16).rearrange("p (k n) -> p k n", k=31) for j in range(2)]
        T_diag = [P.tok("diag%d" % j, AT0 + j * DGB, AT0 + (j + 1) * DGB) for j in range(2)]
        assert 2 * DGB <= 16384

        banks = [es.enter_context(nc.psum_tensor("ps%d" % i, [128, 512], F32)) for i in range(8)]
        T_bank = [P.tok("bank%d" % i) for i in range(8)]
        bptr = [0]

        def bank():
            i = bptr[0] % 8
            bptr[0] += 1
            return banks[i], T_bank[i]

        rptr = {"s": 0, "o": 0}

        def bank_short():
            i = rptr["s"] % 4
            rptr["s"] += 1
            return banks[i], T_bank[i]

        def bank_o():
            i = 4 + rptr["o"] % 2
            rptr["o"] += 1
            return banks[i], T_bank[i]

        smptr = [0]

        def small():
            i = smptr[0] % NSM
            smptr[0] += 1
            return sm[i], T_sm[i]

        cnt = {"xn": 0, "tmp": 0}

        def ncd(fn):
            def f(e):
                with nc.allow_non_contiguous_dma(reason="small strided constant load"):
                    return fn(e)
            return f

        dumped = set()
        cur_g = [0]

        def dump(name, ap, toks):
            if name not in dbg or name in dumped or cur_g[0] != dbg_group:
                return
            dumped.add(name)
            d = nc.dram_tensor("dbg_" + name, list(ap.shape), F32, kind="ExternalOutput").ap()
            P.op("pool", lambda e: e.dma_start(out=d, in_=ap), reads=toks, stream="dbg_" + name)

        def stage(name):
            if stop == name:
                raise _Stop()

        W = [T_const]
        P.op("sp", lambda e: e.dma_start(out=identf[:], in_=c_ident[:, :]), writes=W, stream="c")
        P.op("sp", lambda e: e.dma_start(out=maskb[:], in_=c_mask[:, :]), writes=W, stream="c")
        P.op("sp", lambda e: e.dma_start(out=decb[:], in_=c_dec[:, :]), writes=W, stream="c")
        P.op("sp", ncd(lambda e: e.dma_start(out=gpre[:], in_=g_pre.rearrange("(k p) -> p k", p=128))), writes=W, stream="c")
        P.op("sp", ncd(lambda e: e.dma_start(out=gfpre[:], in_=g_fpre.rearrange("(k p) -> p k", p=128))), writes=W, stream="c")
        P.op("sp", ncd(lambda e: e.dma_start(out=cb[:], in_=dw_b.rearrange("(c p) -> p c", p=128))), writes=W, stream="c")
        P.op("sp", ncd(lambda e: e.dma_start(out=lnw[:], in_=ln_w.rearrange("(c p) -> p c", p=128))), writes=W, stream="c")
        P.op("sp", ncd(lambda e: e.dma_start(out=lnb[:], in_=ln_b.rearrange("(c p) -> p c", p=128))), writes=W, stream="c")
        for c in range(4):
            P.op("sp", ncd(lambda e, c=c: e.dma_start(out=cw[:, c, :], in_=dw_w[:, c * 128:(c + 1) * 128].rearrange("k p -> p k"))),
                 writes=W, stream="c")
        T_c2 = P.tok("const2")
        P.op("dve", lambda e: e.tensor_copy(out=identb[:], in_=identf[:]), reads=W, writes=[T_c2])
        P.op("dve", lambda e: e.memset(ones_s[:], 1.0 / 512), writes=[T_c2])
        P.op("dve", lambda e: e.memset(nhalf[:], -0.5), writes=[T_c2])
        P.op("dve", lambda e: e.memset(epsb[:, 0:4], EPS), writes=[T_c2])
        P.op("dve", lambda e: e.memset(epsb[:, 4:8], 4 * EPS), writes=[T_c2])
        P.op("dve", lambda e: e.tensor_scalar(out=cw[:], in0=cw[:], scalar1=0.5, scalar2=None, op0=ALU.mult),
             reads=W, writes=[T_c2])
        CONST = [T_const, T_c2]

        def rstd_chain(ss_ap, ss_tok, n, scale, width=1, eps4=False):
            v, tv = small()
            eo = 4 if eps4 else 0
            P.op("dve", lambda e: e.scalar_tensor_tensor(out=v[:, 0:width], in0=ss_ap, scalar=scale, in1=epsb[:, eo:eo + width],
                                                         op0=ALU.mult, op1=ALU.add), reads=[ss_tok] + CONST, writes=[tv])
            r, tr = small()
            P.op("pool", lambda e: e.tensor_tensor(out=r[:, 0:width], in0=v[:, 0:width], in1=nhalf[:, 0:width], op=ALU.pow),
                 reads=[tv] + CONST, writes=[tr])
            return r, tr

        def norm_phase(gain_ap):
            rs_ = {}
            evs = {}
            for i in range(11):
                if i < 8:
                    s_, ts = small()
                    P.op("act", lambda e, i=i, s_=s_: e.activation(out=junk[:], in_=xt[:, i, :], func=AF.Square, accum_out=s_[:, 0:1]),
                         reads=[T_xt[i]], writes=[T_junk, ts])
                    v, tv = small()
                    P.op("dve", lambda e, s_=s_, v=v: e.scalar_tensor_tensor(out=v[:, 0:1], in0=s_[:, 0:1], scalar=1.0 / 1024, in1=epsb[:, 0:1],
                                                                             op0=ALU.mult, op1=ALU.add), reads=[ts] + CONST, writes=[tv])
                    r, tr = small()
                    P.op("pool", lambda e, v=v, r=r: e.tensor_tensor(out=r[:, 0:1], in0=v[:, 0:1], in1=nhalf[:, 0:1], op=ALU.pow),
                         reads=[tv] + CONST, writes=[tr])
                    rs_[i] = (r, tr)
                j = i - 2
                if 0 <= j < 8:
                    r, tr = rs_[j]
                    b = cnt["xn"] % 2
                    cnt["xn"] += 1
                    P.op("act", lambda e, j=j, b=b, r=r: e.activation(out=xn[b][:], in_=xt[:, j, :], func=AF.Copy, scale=r[:, 0:1]),
                         reads=[T_xt[j], tr], writes=[T_xn[b]])
                    evs[j] = transposes_to_hT(j, b, gain_ap)
                j = i - 3
                if 0 <= j < 8:
                    evs[j]()

        def transposes_to_hT(i, b, gain_ap):
            ps, tps = bank()
            psb = ps[:].bitcast(BF16)

            def tr(e):
                for k in range(8):
                    ins = e.transpose(out=psb[:, k * 128:(k + 1) * 128], in_=xn[b][:, k * 128:(k + 1) * 128], identity=identb[:])
                return ins
            P.op("pe", tr, reads=[T_xn[b]] + CONST, writes=[tps])
            dst = hT[:, :, i * 128:(i + 1) * 128]
            src = psb.rearrange("p (k t) -> p k t", k=8)

            def evac():
                if gain_ap is not None:
                    P.op("dve", lambda e: e.tensor_tensor(out=dst, in0=src, in1=gain_ap.unsqueeze(2).to_broadcast([128, 8, 128]),
                                                           op=ALU.mult), reads=[tps] + CONST, writes=[T_hT[i]])
                else:
                    P.op("act", lambda e: e.activation(out=dst, in_=src, func=AF.Copy), reads=[tps], writes=[T_hT[i]])
            return evac

        def load_gbc(vec):
            P.op("sp", lambda e: e.dma_start(out=gbc[:], in_=vec.partition_broadcast(128)), writes=[T_gbc], stream="g")

        def post_norm_residual(i, pss, in_is_psum, scale, out_ap=None, out_tok=None, extra_rscale=None):
            s, ts = small()
            for nh_ in range(2):
                src, tsrc = pss[nh_]
                P.op("act", lambda e, src=src, nh_=nh_: e.activation(out=junk[:, 0:512], in_=src, func=AF.Square,
                                                                    accum_out=s[:, nh_:nh_ + 1]),
                     reads=[tsrc], writes=[T_junk, ts])
            s2, ts2 = small()
            P.op("dve", lambda e: e.tensor_tensor(out=s2[:, 0:1], in0=s[:, 0:1], in1=s[:, 1:2], op=ALU.add), reads=[ts], writes=[ts2])
            if extra_rscale is not None:
                assert extra_rscale == 0.5
                r, tr = rstd_chain(s2[:, 0:1], ts2, 1, 4.0 * scale, eps4=True)
            else:
                r, tr = rstd_chain(s2[:, 0:1], ts2, 1, scale)
            for nh_ in range(2):
                src, tsrc = pss[nh_]
                tb = cnt["tmp"] % 4
                cnt["tmp"] += 1
                P.op("dve", lambda e, src=src, tb=tb, nh_=nh_, r=r: e.scalar_tensor_tensor(
                    out=tmp32[tb][:], in0=src, scalar=r[:, 0:1], in1=gbc[:, nh_ * 512:(nh_ + 1) * 512], op0=ALU.mult, op1=ALU.mult),
                    reads=[tsrc, tr, T_gbc], writes=[T_tmp32[tb]])
                if out_ap is None:
                    dst = xt[:, i, nh_ * 512:(nh_ + 1) * 512]
                    P.op("dve", lambda e, dst=dst, tb=tb: e.tensor_tensor(out=dst, in0=dst, in1=tmp32[tb][:], op=ALU.add),
                         reads=[T_tmp32[tb], T_xt[i]], writes=[T_xt[i]])
                else:
                    dst = out_ap[:, nh_ * 512:(nh_ + 1) * 512]
                    P.op("dve", lambda e, dst=dst, tb=tb, nh_=nh_: e.tensor_tensor(
                        out=dst, in0=xt[:, i, nh_ * 512:(nh_ + 1) * 512], in1=tmp32[tb][:], op=ALU.add),
                        reads=[T_tmp32[tb], T_xt[i]], writes=[out_tok])

        w_in_v = w_in.rearrange("(k p) n -> p k n", p=128)
        w_out_v = w_out.rearrange("(k p) n -> p k n", p=128)
        w_gate_v = w_gate.rearrange("(k p) n -> p k n", p=128)
        w_up_v = w_up.rearrange("(k p) n -> p k n", p=128)
        w_down_v = w_down.rearrange("(c p) n -> p c n", p=128)
        w_pg_v = w_pg.rearrange("(k p) n -> p k n", p=128)
        w_pp_v = w_pp.rearrange("(k p) n -> p k n", p=128)

        def load_ring(slot, cc):
            P.op("pool", lambda e: e.dma_start(out=ring[slot][:], in_=w_in_v[:, :, cc * 512:(cc + 1) * 512]),
                 writes=[T_ring[slot]], stream="ring%d" % slot)

        try:
          for g in range(ngroups):
              cur_g[0] = g
              half = g % 2
              tok0 = g * GT
              pos0 = half * GT

              P.op("sp", lambda e, pos0=pos0: e.dma_start(out=cosb[:], in_=c_cos[pos0:pos0 + GT, :].rearrange("(i p) d -> p i d", p=128)),
                   writes=[T_cos], stream="tab0")
              P.op("sp", lambda e, pos0=pos0: e.dma_start(out=sinb[:], in_=c_sin[pos0:pos0 + GT, :].rearrange("(i p) d -> p i d", p=128)),
                   writes=[T_sin], stream="tab1")
              load_ring(0, 0)
              load_ring(1, 1)
              if half == 0:
                  P.op("pool", lambda e: e.memset(S32[:], 0.0), writes=[T_S32])
                  P.op("pool", lambda e: e.memset(Sbf[:], 0.0), writes=[T_Sbf])
                  for c in range(4):
                      P.op("pool", lambda e, c=c: e.memset(glu[:, c, 0:30], 0.0), writes=[T_glu[c][0]])
              else:
                  for c in range(4):
                      P.op("pool", lambda e, c=c: e.tensor_copy(out=glu[:, c, 0:30], in_=halo_sv[:, c, :]),
                           reads=[T_halo], writes=[T_glu[c][0]])

              for i in range(8):
                  P.op("sp", lambda e, i=i, tok0=tok0: e.dma_start(out=xt[:, i, :], in_=x[tok0 + i * 128: tok0 + (i + 1) * 128, :]),
                       writes=[T_xt[i]], stream="x%d" % i)
              norm_phase(gpre[:])

              dump('hT', hT[:], T_hT)
              stage('p1')
              for c in range(4):
                  for th in range(2):
                      psA, tA = bank()
                      psB, tB = bank()

                      def mmab(e, c=c, th=th, psA=psA, psB=psB):
                          for k in range(8):
                              e.matmul(psA[:], lhsT=ring[0][:, k, c * 128:(c + 1) * 128], rhs=hT[:, k, th * 512:(th + 1) * 512],
                                       start=(k == 0), stop=(k == 7))
                          for k in range(8):
                              ins = e.matmul(psB[:], lhsT=ring[1][:, k, c * 128:(c + 1) * 128], rhs=hT[:, k, th * 512:(th + 1) * 512],
                                             start=(k == 0), stop=(k == 7))
                          return ins
                      P.op("pe", mmab, reads=T_ring + T_hT[th * 4:(th + 1) * 4], writes=[tA, tB])
                      tb = (c * 2 + th) % 2
                      th_ap, th_tok = tanh_t[tb]
                      P.op("act", lambda e, psB=psB, th_ap=th_ap: e.activation(out=th_ap, in_=psB[:], func=AF.Tanh, scale=0.5),
                           reads=[tB], writes=[th_tok])
                      P.op("dve", lambda e, c=c, th=th, psA=psA, th_ap=th_ap: e.scalar_tensor_tensor(
                          out=glu[:, c, 30 + th * 512: 30 + (th + 1) * 512], in0=th_ap, scalar=1.0, in1=psA[:],
                          op0=ALU.add, op1=ALU.mult), reads=[tA, th_tok], writes=[T_glu[c][1 + th]])

              for c in range(4):
                  P.op("pool", lambda e, c=c: e.tensor_copy(out=halo_sv[:, c, :], in_=glu[:, c, GT:GT + 30]),
                       reads=[T_glu[c][2]], writes=[T_halo])
              dump('glu', glu, [t for tt in T_glu for t in tt])
              stage('glu')
              load_ring(0, 2)
              load_ring(1, 3)
              for which in range(3):
                  slot = which % 2
                  if which == 2:
                      load_ring(0, 4)
                  for i in range(8):
                      ps, tps = bank()

                      def mmq(e, i=i, ps=ps, slot=slot):
                          for k in range(8):
                              ins = e.matmul(ps[:], lhsT=hT[:, k, i * 128:(i + 1) * 128], rhs=ring[slot][:, k, :],
                                             start=(k == 0), stop=(k == 7))
                          return ins
                      P.op("pe", mmq, reads=[T_ring[slot], T_hT[i]], writes=[tps])
                      if which == 2:
                          P.op("act", lambda e, i=i, ps=ps: e.activation(out=vs[:, i, :], in_=ps[:], func=AF.Copy),
                               reads=[tps], writes=[T_vs[i]])
                          continue
                      tb = i % 2
                      q_ap, q_tok = qsc[tb]
                      for h in range(4):
                          P.op("act", lambda e, h=h, ps=ps, q_ap=q_ap, which=which: e.activation(
                              out=q_ap[:, h * 128:(h + 1) * 128], in_=ps[:, h * 128:(h + 1) * 128], func=AF.Copy,
                              scale=decb[:, which * 4 + h: which * 4 + h + 1]), reads=[tps] + CONST, writes=[q_tok])
                      t1_ap, t1_tok = rt1[tb]
                      t2_ap, t2_tok = rt2[tb]
                      q3 = q_ap.rearrange("p (h d) -> p h d", h=4)
                      t13 = t1_ap.rearrange("p (h d) -> p h d", h=4)
                      t23 = t2_ap.rearrange("p (h d) -> p h d", h=4)
                      P.op("dve", lambda e, i=i, q3=q3, t13=t13: e.tensor_tensor(
                          out=t13, in0=q3, in1=cosb[:, i, :].unsqueeze(1).to_broadcast([128, 4, 128]), op=ALU.mult),
                          reads=[q_tok, T_cos], writes=[t1_tok])
                      P.op("pool", lambda e, i=i, q3=q3, t23=t23: e.tensor_tensor(
                          out=t23[:, :, 0:64], in0=q3[:, :, 64:128], in1=sinb[:, i, 0:64].unsqueeze(1).to_broadcast([128, 4, 64]),
                          op=ALU.mult), reads=[q_tok, T_sin], writes=[t2_tok])
                      P.op("pool", lambda e, i=i, q3=q3, t23=t23: e.tensor_tensor(
                          out=t23[:, :, 64:128], in0=q3[:, :, 0:64], in1=sinb[:, i, 64:128].unsqueeze(1).to_broadcast([128, 4, 64]),
                          op=ALU.mult), reads=[q_tok, T_sin], writes=[t2_tok])
                      dst = (qs if which == 0 else ks)[:, i, :]
                      dtok = (T_qs if which == 0 else T_ks)[i]
                      P.op("dve", lambda e, dst=dst, t1_ap=t1_ap, t2_ap=t2_ap: e.tensor_tensor(out=dst, in0=t1_ap, in1=t2_ap, op=ALU.add),
                           reads=[t1_tok, t2_tok], writes=[dtok])
              load_ring(1, 5)

              dump('qs', qs, T_qs)
              dump('ks', ks, T_ks)
              dump('vs', vs, T_vs)
              stage('qkv')
              conv_blocks = [(th, c) for th in range(2) for c in range(4)]

              def diag_build(bi):
                  th, c = conv_blocks[bi]
                  j = bi % 2
                  P.op("dve", lambda e, c=c, j=j: e.tensor_tensor(out=diag[j], in0=identf[:].unsqueeze(1).to_broadcast([128, 31, 128]),
                                                                 in1=cw[:, c, :].unsqueeze(2).to_broadcast([128, 31, 128]), op=ALU.mult),
                       reads=CONST, writes=[T_diag[j]])

              def conv_part(bi, part):
                  th, c = conv_blocks[bi]
                  j = bi % 2
                  cps, tcps = banks[6 + j], T_bank[6 + j]
                  k0, k1 = [(0, 10), (10, 20), (20, 31)][part]
                  rtoks = [T_glu[c][1 + th]] + ([T_glu[c][0]] if th == 0 else [T_glu[c][1]])

                  def mmc(e):
                      for k in range(k0, k1):
                          ins = e.matmul(cps[:], lhsT=diag[j][:, k, :], rhs=glu[:, c, th * 512 + k: th * 512 + k + 512],
                                         start=(k == 0), stop=(k == 30))
                      return ins
                  P.op("pe", mmc, reads=rtoks + [T_diag[j]], writes=[tcps])
                  if part == 2:
                      P.op("act", lambda e: e.activation(out=acc[c], in_=cps[:], func=AF.Identity, bias=cb[:, c:c + 1]),
                           reads=[tcps] + CONST, writes=[T_acc[c]])
                      P.op("act", lambda e: e.activation(out=ybf[:, c, :], in_=cps[:], func=AF.Identity, bias=cb[:, c:c + 1]),
                           reads=[tcps] + CONST, writes=[T_ybf[c]])
                      P.op("act", lambda e: e.activation(out=ysq[:, c, :], in_=cps[:], func=AF.Square, bias=cb[:, c:c + 1]),
                           reads=[tcps] + CONST, writes=[T_ysq[c]])
                      if c == 3:
                          conv_finish(th)
                  if part == 1 and bi + 1 < len(conv_blocks):
                      diag_build(bi + 1)

              def conv_finish(th):
                  psM, tM = bank_short()
                  psQ, tQ = bank_short()

                  def mmst(e):
                      for c in range(4):
                          e.matmul(psM[:], lhsT=ones_s[:], rhs=ybf[:, c, :], start=(c == 0), stop=(c == 3))
                      for c in range(4):
                          ins = e.matmul(psQ[:], lhsT=ones_s[:], rhs=ysq[:, c, :], start=(c == 0), stop=(c == 3))
                      return ins
                  P.op("pe", mmst, reads=T_ybf + T_ysq + CONST, writes=[tM, tQ])
                  P.op("act", lambda e: e.activation(out=mu_sb, in_=psM[:], func=AF.Copy), reads=[tM], writes=[T_mu])
                  P.op("dve", lambda e: e.tensor_tensor(out=musq, in0=mu_sb, in1=mu_sb, op=ALU.mult), reads=[T_mu], writes=[T_musq])
                  P.op("dve", lambda e: e.scalar_tensor_tensor(out=musq, in0=psQ[:], scalar=EPS, in1=musq, op0=ALU.add, op1=ALU.subtract),
                       reads=[tQ, T_musq], writes=[T_musq])
                  P.op("act", lambda e: e.activation(out=rstdb, in_=musq, func=AF.Sqrt), reads=[T_musq], writes=[T_rstdb])
                  P.op("dve", lambda e: e.reciprocal(out=rstdb, in_=rstdb), reads=[T_rstdb], writes=[T_rstdb])
                  for c in range(4):
                      P.op("dve", lambda e, c=c: e.tensor_tensor(out=acc[c], in0=acc[c], in1=mu_sb, op=ALU.subtract),
                           reads=[T_acc[c], T_mu], writes=[T_acc[c]])
                  for c in range(4):
                      P.op("dve", lambda e, c=c: e.tensor_tensor(out=acc[c], in0=acc[c], in1=rstdb, op=ALU.mult),
                           reads=[T_acc[c], T_rstdb], writes=[T_acc[c]])
                  for c in range(4):
                      P.op("act", lambda e, c=c: e.activation(out=catT[:, c, th * 512:(th + 1) * 512], in_=acc[c], func=AF.Silu,
                                                              scale=lnw[:, c:c + 1], bias=lnb[:, c:c + 1]),
                           reads=[T_acc[c]] + CONST, writes=T_cat[c][th * 4:(th + 1) * 4])

              ctx = {}

              def ret_A(i):
                  tb = i % 2
                  psg, tpsg = bank_short()

                  def mmg(e):
                      for k in range(8):
                          ins = e.matmul(psg[:], lhsT=hT[:, k, i * 128:(i + 1) * 128], rhs=ring[1][:, k, :], start=(k == 0), stop=(k == 7))
                      return ins
                  P.op("pe", mmg, reads=[T_ring[1], T_hT[i]], writes=[tpsg])
                  sg_ap, sg_tok = sgb[tb]
                  P.op("act", lambda e: e.activation(out=sg_ap, in_=psg[:], func=AF.Silu), reads=[tpsg], writes=[sg_tok])
                  pst, tpst = bank_short()
                  pstb = pst[:].bitcast(BF16)

                  def trqk(e):
                      for h in range(4):
                          e.transpose(out=pstb[:, h * 128:(h + 1) * 128], in_=qs[:, i, h * 128:(h + 1) * 128], identity=identb[:])
                      for h in range(4):
                          ins = e.transpose(out=pstb[:, 512 + h * 128: 512 + (h + 1) * 128], in_=ks[:, i, h * 128:(h + 1) * 128], identity=identb[:])
                      return ins
                  P.op("pe", trqk, reads=[T_qs[i], T_ks[i]] + CONST, writes=[tpst])
                  qk_ap, qk_tok = qkT[tb]
                  P.op("dve", lambda e: e.tensor_copy(out=qk_ap, in_=pstb), reads=[tpst], writes=[qk_tok])
                  ctx[i] = dict(sg=(sg_ap, sg_tok), qk=(qk_ap, qk_tok))

              def ret_A2(i):
                  tb = i % 2
                  qk_ap, qk_tok = ctx[i]["qk"]
                  pss, tpss = bank_short()

                  def mms(e):
                      for h in range(4):
                          ins = e.matmul(pss[:, h * 128:(h + 1) * 128], lhsT=qk_ap[:, 512 + h * 128: 512 + (h + 1) * 128],
                                         rhs=qk_ap[:, h * 128:(h + 1) * 128], start=True, stop=True)
                      return ins
                  P.op("pe", mms, reads=[qk_tok], writes=[tpss])
                  sT_ap, sT_tok = sTb[tb]
                  P.op("dve", lambda e: e.tensor_tensor(out=sT_ap, in0=pss[:], in1=maskb[:], op=ALU.mult),
                       reads=[tpss] + CONST, writes=[sT_tok])
                  ctx[i]["sT"] = (sT_ap, sT_tok)

              def ret_B1(i):
                  qk_ap, qk_tok = ctx[i]["qk"]
                  sT_ap, sT_tok = ctx[i]["sT"]
                  pso, tpso = bank_o()

                  def mmo(e):
                      for h in range(4):
                          e.matmul(pso[:, h * 128:(h + 1) * 128], lhsT=sT_ap[:, h * 128:(h + 1) * 128], rhs=vs[:, i, h * 128:(h + 1) * 128],
                                   start=True, stop=False)
                          ins = e.matmul(pso[:, h * 128:(h + 1) * 128], lhsT=qk_ap[:, h * 128:(h + 1) * 128], rhs=Sbf[:, h * 128:(h + 1) * 128],
                                         start=False, stop=True)
                      return ins
                  P.op("pe", mmo, reads=[sT_tok, T_vs[i], qk_tok, T_Sbf], writes=[tpso])
                  pkv, tpkv = bank_short()

                  def mmkv(e):
                      for h in range(4):
                          ins = e.matmul(pkv[:, h * 128:(h + 1) * 128], lhsT=ks[:, i, h * 128:(h + 1) * 128], rhs=vs[:, i, h * 128:(h + 1) * 128],
                                         start=True, stop=True)
                      return ins
                  P.op("pe", mmkv, reads=[T_ks[i], T_vs[i]], writes=[tpkv])
                  for h in range(4):
                      P.op("dve", lambda e, h=h: e.scalar_tensor_tensor(
                          out=S32[:, h * 128:(h + 1) * 128], in0=S32[:, h * 128:(h + 1) * 128], scalar=g128[h],
                          in1=pkv[:, h * 128:(h + 1) * 128], op0=ALU.mult, op1=ALU.add), reads=[tpkv, T_S32], writes=[T_S32])
                  P.op("act", lambda e: e.activation(out=Sbf[:], in_=S32[:], func=AF.Copy), reads=[T_S32], writes=[T_Sbf])
                  so, tso = small()
                  for h in range(4):
                      P.op("act", lambda e, h=h: e.activation(out=junk[:, 0:128], in_=pso[:, h * 128:(h + 1) * 128],
                                                              func=AF.Square, accum_out=so[:, h:h + 1]),
                           reads=[tpso], writes=[T_junk, tso])
                  r4, tr4 = rstd_chain(so[:, 0:4], tso, 4, 1.0 / 128, width=4)
                  ctx[i]["o"] = (pso, tpso, r4, tr4)

              def ret_B2(i):
                  tb = i % 2
                  pso, tpso, r4, tr4 = ctx[i]["o"]
                  sg_ap, sg_tok = ctx[i]["sg"]
                  yr_ap, yr_tok = yrb[tb]
                  for h in range(4):
                      P.op("dve", lambda e, h=h: e.scalar_tensor_tensor(
                          out=yr_ap[:, h * 128:(h + 1) * 128], in0=pso[:, h * 128:(h + 1) * 128], scalar=r4[:, h:h + 1],
                          in1=sg_ap[:, h * 128:(h + 1) * 128], op0=ALU.mult, op1=ALU.mult), reads=[tpso, tr4, sg_tok], writes=[yr_tok])
                  ctx[i]["yr"] = (yr_ap, yr_tok)

              def ret_B3(i):
                  yr_ap, yr_tok = ctx[i]["yr"]
                  psy, tpsy = bank_short()
                  psyb = psy[:].bitcast(BF16)

                  def try_(e):
                      for h in range(4):
                          ins = e.transpose(out=psyb[:, h * 128:(h + 1) * 128], in_=yr_ap[:, h * 128:(h + 1) * 128], identity=identb[:])
                      return ins
                  P.op("pe", try_, reads=[yr_tok] + CONST, writes=[tpsy])
                  P.op("act", lambda e: e.activation(out=catT[:, 4:8, i * 128:(i + 1) * 128],
                                                     in_=psyb[:, 0:512].rearrange("p (h t) -> p h t", h=4), func=AF.Copy),
                       reads=[tpsy], writes=[T_cat[4 + h][i] for h in range(4)])

              diag_build(0)
              ret_A(0)
              ret_A2(0)
              for i in range(8):
                  conv_part(i, 0)
                  if i + 1 < 8:
                      ret_A(i + 1)
                  ret_B1(i)
                  conv_part(i, 1)
                  if i + 1 < 8:
                      ret_A2(i + 1)
                  ret_B2(i)
                  if i >= 1:
                      ret_B3(i - 1)
                  conv_part(i, 2)
              ret_B3(7)

              dump('catT', catT, [t for tt in T_cat for t in tt])
              stage('cat')
              P.op("pool", lambda e: e.dma_start(out=wo[:], in_=w_out_v), writes=[T_wo], stream="wo")
              load_gbc(g_post)
              for i in range(8):
                  pss2 = []
                  for nh_ in range(2):
                      ps, tps = bank()

                      def mmwo(e, i=i, nh_=nh_, ps=ps):
                          for k in range(8):
                              ins = e.matmul(ps[:], lhsT=catT[:, k, i * 128:(i + 1) * 128], rhs=wo[:, k, nh_ * 512:(nh_ + 1) * 512],
                                             start=(k == 0), stop=(k == 7))
                          return ins
                      P.op("pe", mmwo, reads=[T_wo] + [T_cat[k][i] for k in range(8)], writes=[tps])
                      pss2.append((ps[:], tps))
                  post_norm_residual(i, pss2, True, 1.0 / 1024)

              dump('x1', xt[:], T_xt)
              stage('x1')
              for j in range(2):
                  P.op("pool", lambda e, j=j: e.dma_start(out=wd[:, j * 11:(j + 1) * 11, :], in_=w_down_v[:, j * 11:(j + 1) * 11, :]),
                       writes=[T_wd[j]], stream="wd%d" % j)
              norm_phase(gfpre[:])
              load_gbc(g_fpost)
              for st in range(11):
                  slot = st % 2
                  P.op("pool", lambda e, st=st, slot=slot: e.dma_start(out=wgr[slot][:], in_=w_gate_v[:, :, st * 256:(st + 1) * 256]),
                       writes=[T_wgr[slot]], stream="wg%d" % slot)
                  P.op("pool", lambda e, st=st, slot=slot: e.dma_start(out=wur[slot][:], in_=w_up_v[:, :, st * 256:(st + 1) * 256]),
                       writes=[T_wur[slot]], stream="wu%d" % slot)
                  for sub in range(2):
                      c = st * 2 + sub
                      for th in range(2):
                          psG, tG = bank()
                          psU, tU = bank()

                          def mmgu(e, sub=sub, th=th, slot=slot, psG=psG, psU=psU):
                              for k in range(8):
                                  e.matmul(psG[:], lhsT=wgr[slot][:, k, sub * 128:(sub + 1) * 128], rhs=hT[:, k, th * 512:(th + 1) * 512],
                                           start=(k == 0), stop=(k == 7))
                              for k in range(8):
                                  ins = e.matmul(psU[:], lhsT=wur[slot][:, k, sub * 128:(sub + 1) * 128], rhs=hT[:, k, th * 512:(th + 1) * 512],
                                                 start=(k == 0), stop=(k == 7))
                              return ins
                          P.op("pe", mmgu, reads=[T_wgr[slot], T_wur[slot]] + T_hT[th * 4:(th + 1) * 4], writes=[tG, tU])
                          tb = th
                          P.op("act", lambda e, psG=psG, tb=tb: e.activation(out=sgt[tb], in_=psG[:], func=AF.Silu), reads=[tG], writes=[T_sgt[tb]])
                          P.op("dve", lambda e, c=c, th=th, psU=psU, tb=tb: e.tensor_tensor(
                              out=aT[:, c, th * 512:(th + 1) * 512], in0=psU[:], in1=sgt[tb], op=ALU.mult),
                              reads=[tU, T_sgt[tb]], writes=[T_a[c][th]])
              for i in range(8):
                  pss2 = []
                  for nh_ in range(2):
                      ps, tps = bank()

                      def mmd(e, i=i, nh_=nh_, ps=ps):
                          for c in range(NFC):
                              ins = e.matmul(ps[:], lhsT=aT[:, c, i * 128:(i + 1) * 128], rhs=wd[:, c, nh_ * 512:(nh_ + 1) * 512],
                                             start=(c == 0), stop=(c == NFC - 1))
                          return ins
                      P.op("pe", mmd, reads=T_wd + [T_a[c][i // 4] for c in range(NFC)], writes=[tps])
                      pss2.append((ps[:], tps))
                  post_norm_residual(i, pss2, True, 1.0 / 1024)

              dump('x2', xt[:], T_xt)
              stage('x2')
              def load_p(i):
                  pb = i % 2
                  P.op("sp", lambda e, i=i, pb=pb, tok0=tok0: e.dma_start(out=pt32[pb], in_=p[tok0 + i * 128: tok0 + (i + 1) * 128, :]),
                       writes=[T_pt32[pb]], stream="p%d" % pb)
              load_p(0)
              load_p(1)
              P.op("pool", lambda e: e.dma_start(out=wpg[:], in_=w_pg_v), writes=[T_wpg], stream="wpg")
              P.op("pool", lambda e: e.dma_start(out=wpp[:], in_=w_pp_v), writes=[T_wpp], stream="wpp")
              load_gbc(g_ple)
              pend = None
              for i in range(9):
                  nxt = None
                  if i < 8:
                      b = cnt["xn"] % 2
                      cnt["xn"] += 1
                      P.op("act", lambda e, i=i, b=b: e.activation(out=xn[b][:], in_=xt[:, i, :], func=AF.Copy), reads=[T_xt[i]], writes=[T_xn[b]])
                      ev1 = transposes_to_hT(i, b, None)
                      pb = i % 2
                      P.op("act", lambda e, pb=pb: e.activation(out=ptb[pb], in_=pt32[pb], func=AF.Copy), reads=[T_pt32[pb]], writes=[T_ptb[pb]])
                      if i + 2 < 8:
                          load_p(i + 2)
                      psp, tpsp = bank()
                      pspb = psp[:].bitcast(BF16)

                      def trp(e, pb=pb, pspb=pspb):
                          for k in range(2):
                              ins = e.transpose(out=pspb[:, k * 128:(k + 1) * 128], in_=ptb[pb][:, k * 128:(k + 1) * 128], identity=identb[:])
                          return ins
                      P.op("pe", trp, reads=[T_ptb[pb]] + CONST, writes=[tpsp])

                      def nxt(i=i, pspb=pspb, tpsp=tpsp, ev1=ev1):
                          ev1()
                          P.op("act", lambda e: e.activation(out=pT[:, :, i * 128:(i + 1) * 128],
                                                             in_=pspb[:, 0:256].rearrange("p (k t) -> p k t", k=2), func=AF.Copy),
                               reads=[tpsp], writes=T_pT[i])
                  if pend is not None:
                      pend()
                  pend = nxt
              for i in range(8):
                  eb = i % 2
                  for nh_ in range(2):
                      psg2, tg2 = bank()
                      psp2, tp2 = bank()

                      def mmple(e, i=i, nh_=nh_, psg2=psg2, psp2=psp2):
                          for k in range(8):
                              e.matmul(psg2[:], lhsT=hT[:, k, i * 128:(i + 1) * 128], rhs=wpg[:, k, nh_ * 512:(nh_ + 1) * 512],
                                       start=(k == 0), stop=(k == 7))
                          for k in range(2):
                              ins = e.matmul(psp2[:], lhsT=pT[:, k, i * 128:(i + 1) * 128], rhs=wpp[:, k, nh_ * 512:(nh_ + 1) * 512],
                                             start=(k == 0), stop=(k == 1))
                          return ins
                      P.op("pe", mmple, reads=[T_hT[i], T_wpg, T_wpp] + T_pT[i], writes=[tg2, tp2])
                      P.op("act", lambda e, nh_=nh_, psg2=psg2, eb=eb: e.activation(out=tt32[eb][:, nh_ * 512:(nh_ + 1) * 512], in_=psg2[:],
                                                                                   func=AF.Tanh, scale=0.5), reads=[tg2], writes=[T_tt32[eb][nh_]])
                      P.op("dve", lambda e, nh_=nh_, psp2=psp2, eb=eb: e.scalar_tensor_tensor(
                          out=e32[eb][:, nh_ * 512:(nh_ + 1) * 512], in0=tt32[eb][:, nh_ * 512:(nh_ + 1) * 512], scalar=1.0, in1=psp2[:],
                          op0=ALU.add, op1=ALU.mult), reads=[tp2, T_tt32[eb][nh_]], writes=[T_e32[eb][nh_]])
                  ob = i % 2
                  post_norm_residual(i, [(e32[eb][:, 0:512], T_e32[eb][0]), (e32[eb][:, 512:1024], T_e32[eb][1])], False, 1.0 / 4096,
                                     out_ap=ot[ob], out_tok=T_ot[ob], extra_rscale=0.5)
                  P.op("sp", lambda e, i=i, ob=ob, tok0=tok0: e.dma_start(out=out[tok0 + i * 128: tok0 + (i + 1) * 128, :], in_=ot[ob]),
                       reads=[T_ot[ob]], stream="out%d" % ob)

              if True:
                  dump('x2T', hT[:], T_hT)
                  dump('pT', pT, [t for tt in T_pT for t in tt])
                  dump('e32', e32[1], T_e32[1])
                  dump('tt32', tt32[1], T_tt32[1])
        except _Stop:
            pass
        P.emit(nc, es)
    return nc


def _constants():
    half = 64
    freqs = 10000.0 ** (-np.arange(half, dtype=np.float64) / half)
    pos = np.arange(2048, dtype=np.float64)
    ang = pos[:, None] * freqs[None, :]
    cos = np.cos(ang).astype(np.float32)
    sin = np.sin(ang).astype(np.float32)
    c_cos = np.concatenate([cos, cos], axis=1)
    c_sin = np.concatenate([-sin, sin], axis=1)
    gam = 1.0 - 2.0 ** (-5.0 - np.arange(4, dtype=np.float64))
    idx = np.arange(128, dtype=np.float64)
    dq = gam[None, :] ** (idx[:, None] + 1.0)
    dk = gam[None, :] ** (127.0 - idx[:, None]) * (128.0 ** -0.5)
    c_dec = np.concatenate([dq, dk], axis=1).astype(np.float32)
    causal = (idx[None, :] >= idx[:, None]).astype(np.float64)
    c_mask = np.stack([causal * gam[h] ** (-128.0) for h in range(4)], axis=1).reshape(128, 512).astype(np.float32)
    return {
        "c_ident": np.eye(128, dtype=np.float32),
        "c_cos": np.ascontiguousarray(c_cos, dtype=np.float32),
        "c_sin": np.ascontiguousarray(c_sin, dtype=np.float32),
        "c_dec": np.ascontiguousarray(c_dec),
        "c_mask": np.ascontiguousarray(c_mask),
    }


def make_in_maps(x, p, mix_pre_norm, w_in, conv_dw_w, conv_dw_b, conv_ln_w, conv_ln_b,
                 w_out, mix_post_norm, ffn_pre_norm, w_ffn_gate, w_ffn_up, w_ffn_down,
                 ffn_post_norm, w_ple_gate, w_ple_proj, ple_post_norm):
    f = lambda a: np.ascontiguousarray(np.asarray(a, dtype=np.float32))
    shared = {
        "w_in": f(w_in[0]), "w_out": f(w_out[0]), "w_gate": f(w_ffn_gate[0]), "w_up": f(w_ffn_up[0]),
        "w_down": f(w_ffn_down[0]), "w_pg": f(w_ple_gate[0]), "w_pp": f(w_ple_proj[0]),
        "g_pre": f(mix_pre_norm[0]), "g_post": f(mix_post_norm[0]), "g_fpre": f(ffn_pre_norm[0]),
        "g_fpost": f(ffn_post_norm[0]), "g_ple": f(ple_post_norm[0]),
        "dw_w": f(conv_dw_w[0]), "dw_b": f(conv_dw_b[0]), "ln_w": f(conv_ln_w[0]), "ln_b": f(conv_ln_b[0]),
    }
    shared.update(_constants())
    xs = f(x).reshape(NCORES, TOK_CORE, 1024)
    ps = f(p[0]).reshape(NCORES, TOK_CORE, 256)
    in_maps = []
    for c in range(NCORES):
        m = dict(shared)
        m["x"] = xs[c]
        m["p"] = ps[c]
        in_maps.append(m)
    return in_maps


def kernel(**inputs):
    in_maps = make_in_maps(**inputs)
    nc = build_program()
    res = run_bass_kernel_spmd(nc, in_maps, core_ids=list(range(NCORES)))
    outs = [np.asarray(r["out"], dtype=np.float32) for r in res.results]
    return np.stack(outs, axis=0).reshape(16, 2048, 1024)
```
